# Optimizing a Trainium2 kernel written in Bass

```python
import math
import jax, jax.numpy as jnp
from jax import lax
import numpy as np

D_MODEL = 4096
BATCH = 2
SEQ = 4096
DEPTH = 2

N_EVEN = (DEPTH + 1) // 2
N_ODD = DEPTH // 2
N_MOD = 6
ADA_INIT_SCALE = 0.5
NORM_EPS = 1e-6

CONV_CH = D_MODEL // 2
CONV_WIDTH = 31
DIFF_HEADS = 8
DIFF_HEAD_DIM = D_MODEL // 2 // (2 * DIFF_HEADS)
DIFF_V_DIM = 2 * DIFF_HEAD_DIM
DIFF_QK = 2 * DIFF_HEADS * DIFF_HEAD_DIM
DIFF_V = DIFF_HEADS * DIFF_V_DIM
DIFF_SUBLN_EPS = 1e-5
Q_BLOCK = 128
ROPE_THETA = 10000.0
EVEN_IN = 2 * CONV_CH + 2 * DIFF_QK + DIFF_V
EVEN_MIX = CONV_CH + DIFF_V

GLA_HEADS = 4
GLA_DK = D_MODEL // 2 // GLA_HEADS
GLA_DV = D_MODEL // GLA_HEADS
GLA_RANK = 16
GLA_TAU = 16.0
GLA_CHUNK = 64
GLA_K = GLA_HEADS * GLA_DK
GLA_V = GLA_HEADS * GLA_DV
ODD_IN = 2 * GLA_K + 2 * GLA_V + GLA_RANK

D_FF = ((8 * D_MODEL // 3 + 255) // 256) * 256
N_EXPERTS = 8
TOP_K = 2
D_FF_EXPERT = D_MODEL

kernel_name = "hybrid_conv_diffattn_gla_moe_adaln"


def rms_norm(x, g, eps=NORM_EPS):
    xf = x.astype(jnp.float32)
    y = xf * lax.rsqrt(jnp.mean(xf * xf, axis=-1, keepdims=True) + eps)
    return (y * g).astype(x.dtype)


def layer_norm(x, g, b, eps=NORM_EPS):
    xf = x.astype(jnp.float32)
    mu = jnp.mean(xf, axis=-1, keepdims=True)
    var = jnp.mean(jnp.square(xf - mu), axis=-1, keepdims=True)
    return ((xf - mu) * lax.rsqrt(var + eps) * g + b).astype(x.dtype)


def modulate(h, shift, scale):
    return h * (1.0 + scale[:, None, :]) + shift[:, None, :]


def rope(x, positions):
    d = x.shape[-1]
    inv = ROPE_THETA ** (-jnp.arange(0, d, 2, dtype=jnp.float32) / d)
    ang = positions.astype(jnp.float32)[:, None] * inv[None, :]
    cos, sin = jnp.cos(ang)[None, :, None, :], jnp.sin(ang)[None, :, None, :]
    xf = x.astype(jnp.float32)
    x1, x2 = xf[..., : d // 2], xf[..., d // 2:]
    return jnp.concatenate([x1 * cos - x2 * sin, x2 * cos + x1 * sin], axis=-1).astype(x.dtype)


def conformer_conv(a_val, a_gate, conv_w, conv_b, ln_g, ln_b):
    u = a_val * jax.nn.sigmoid(a_gate)
    u = lax.conv_general_dilated(
        u, conv_w[:, None, :].astype(u.dtype), window_strides=(1,),
        padding=[(CONV_WIDTH - 1, 0)], dimension_numbers=("NWC", "WIO", "NWC"),
        feature_group_count=CONV_CH) + conv_b
    return jax.nn.silu(layer_norm(u, ln_g, ln_b))


def diff_attention(q, k, v, lam, subln_g, lambda_init):
    bsz, h2, s, d = q.shape
    h = h2 // 2
    nb = s // Q_BLOCK
    qb = q.reshape(bsz, h2, nb, Q_BLOCK, d).transpose(2, 0, 1, 3, 4)
    kpos = jnp.arange(s)
    scale = d ** -0.5

    def block(args):
        qi, i = args
        sc = jnp.einsum("bhqd,bhkd->bhqk", qi, k).astype(jnp.float32) * scale
        qpos = i * Q_BLOCK + jnp.arange(Q_BLOCK)
        sc = jnp.where(kpos[None, :] <= qpos[:, None], sc, -jnp.inf)
        p = jax.nn.softmax(sc, axis=-1).reshape(bsz, h, 2, Q_BLOCK, s)
        a = p[:, :, 0] - lam * p[:, :, 1]
        return jnp.einsum("bhqk,bhkv->bhqv", a.astype(v.dtype), v)

    o = lax.map(block, (qb, jnp.arange(nb)))
    o = o.transpose(1, 2, 0, 3, 4).reshape(bsz, h, s, v.shape[-1])
    o = rms_norm(o, subln_g, DIFF_SUBLN_EPS) * (1.0 - lambda_init)
    return o.transpose(0, 2, 1, 3).reshape(bsz, s, h * v.shape[-1])


def gla_chunked(q, k, v, log_a):
    bsz, h, s, dk = q.shape
    dv = v.shape[-1]
    n = s // GLA_CHUNK
    f32 = jnp.float32
    q, k, log_a = [t.astype(f32).reshape(bsz, h, n, GLA_CHUNK, dk) for t in (q, k, log_a)]
    v = v.astype(f32).reshape(bsz, h, n, GLA_CHUNK, dv)
    b = jnp.cumsum(log_a, axis=3)
    b_last = b[:, :, :, -1:, :]
    q_t = q * jnp.exp(b)
    k_t = k * jnp.exp(-b)
    k_dec = k * jnp.exp(b_last - b)
    causal = jnp.tril(jnp.ones((GLA_CHUNK, GLA_CHUNK), dtype=bool))
    attn = jnp.where(causal, jnp.einsum("bhnid,bhnjd->bhnij", q_t, k_t), 0.0)
    o_intra = jnp.einsum("bhnij,bhnjv->bhniv", attn, v)

    def step(state, inp):
        qc, kc, vc, dc = inp
        o = jnp.einsum("bhid,bhdv->bhiv", qc, state)
        state = dc[..., None] * state + jnp.einsum("bhid,bhiv->bhdv", kc, vc)
        return state, o

    xs = (jnp.moveaxis(q_t, 2, 0), jnp.moveaxis(k_dec, 2, 0), jnp.moveaxis(v, 2, 0),
          jnp.moveaxis(jnp.exp(b_last[:, :, :, 0, :]), 2, 0))
    _, o_inter = lax.scan(step, jnp.zeros((bsz, h, dk, dv), f32), xs)
    o = o_intra + jnp.moveaxis(o_inter, 0, 2)
    return o.reshape(bsz, h, s, dv)


def swiglu(h, w_gate, w_up, w_down):
    return (jax.nn.silu(h @ w_gate) * (h @ w_up)) @ w_down


def moe_swiglu(h, w_router, w_gate, w_up, w_down):
    bsz, s, d = h.shape
    t = h.reshape(bsz * s, d)
    logits = (t @ w_router).astype(jnp.float32)
    top_v, top_i = lax.top_k(logits, TOP_K)
    weights = jax.nn.softmax(top_v, axis=-1)
    gates = jnp.sum(jax.nn.one_hot(top_i, N_EXPERTS, dtype=jnp.float32) * weights[..., None], axis=1)
    out = jnp.zeros((bsz * s, d), dtype=jnp.float32)
    for e in range(N_EXPERTS):
        out = out + gates[:, e, None] * swiglu(t, w_gate[e], w_up[e], w_down[e]).astype(jnp.float32)
    return out.astype(h.dtype).reshape(bsz, s, d)


def _normal(key, shape, scale):
    return jax.random.normal(key, shape, jnp.float32) * scale


def setup_inputs(seed: int = 0) -> dict:
    key = jax.random.key(seed)
    ks = jax.random.split(key, 32)
    D = D_MODEL
    return {
        "x": _normal(ks[0], (BATCH, SEQ, D), 1.0),
        "c": _normal(ks[1], (BATCH, D), 1.0),
        "norm_gains": 1.0 + _normal(ks[2], (DEPTH, 2, D), 0.02),
        "ada_w": _normal(ks[3], (DEPTH, D, N_MOD * D), ADA_INIT_SCALE * D ** -0.5),
        "ada_b": _normal(ks[4], (DEPTH, N_MOD * D), 0.02),
        "e_w_in": _normal(ks[5], (N_EVEN, D, EVEN_IN), D ** -0.5),
        "e_conv_w": _normal(ks[6], (N_EVEN, CONV_WIDTH, CONV_CH), CONV_WIDTH ** -0.5),
        "e_conv_b": _normal(ks[7], (N_EVEN, CONV_CH), 0.02),
        "e_conv_ln_g": 1.0 + _normal(ks[8], (N_EVEN, CONV_CH), 0.02),
        "e_conv_ln_b": _normal(ks[9], (N_EVEN, CONV_CH), 0.02),
        "e_diff_lambda": _normal(ks[10], (N_EVEN, 4, DIFF_HEAD_DIM), 0.1),
        "e_diff_subln": 1.0 + _normal(ks[11], (N_EVEN, DIFF_V_DIM), 0.02),
        "e_w_out": _normal(ks[12], (N_EVEN, EVEN_MIX, D), EVEN_MIX ** -0.5),
        "e_ffn_gate": _normal(ks[13], (N_EVEN, D, D_FF), D ** -0.5),
        "e_ffn_up": _normal(ks[14], (N_EVEN, D, D_FF), D ** -0.5),
        "e_ffn_down": _normal(ks[15], (N_EVEN, D_FF, D), D_FF ** -0.5),
        "o_w_in": _normal(ks[16], (N_ODD, D, ODD_IN), D ** -0.5),
        "o_gate_w2": _normal(ks[17], (N_ODD, GLA_RANK, GLA_K), GLA_RANK ** -0.5),
        "o_gate_b": _normal(ks[18], (N_ODD, GLA_K), 0.02),
        "o_gla_norm": 1.0 + _normal(ks[19], (N_ODD, GLA_DV), 0.02),
        "o_w_out": _normal(ks[20], (N_ODD, GLA_V, D), GLA_V ** -0.5),
        "o_router": _normal(ks[21], (N_ODD, D, N_EXPERTS), D ** -0.5),
        "o_exp_gate": _normal(ks[22], (N_ODD, N_EXPERTS, D, D_FF_EXPERT), D ** -0.5),
        "o_exp_up": _normal(ks[23], (N_ODD, N_EXPERTS, D, D_FF_EXPERT), D ** -0.5),
        "o_exp_down": _normal(ks[24], (N_ODD, N_EXPERTS, D_FF_EXPERT, D), D_FF_EXPERT ** -0.5),
        "final_norm": 1.0 + _normal(ks[25], (D,), 0.02),
    }


def reference(x, c, norm_gains, ada_w, ada_b, e_w_in, e_conv_w, e_conv_b, e_conv_ln_g,
              e_conv_ln_b, e_diff_lambda, e_diff_subln, e_w_out, e_ffn_gate, e_ffn_up,
              e_ffn_down, o_w_in, o_gate_w2, o_gate_b, o_gla_norm, o_w_out, o_router,
              o_exp_gate, o_exp_up, o_exp_down, final_norm):
    bsz, s, _ = x.shape
    positions = jnp.arange(s)
    c_act = jax.nn.silu(c)
    for layer in range(DEPTH):
        mod = c_act @ ada_w[layer] + ada_b[layer]
        sh_m, sc_m, g_m, sh_f, sc_f, g_f = jnp.split(mod, N_MOD, axis=-1)
        h = modulate(rms_norm(x, norm_gains[layer, 0]), sh_m, sc_m)
        if layer % 2 == 0:
            i = layer // 2
            proj = h @ e_w_in[i]
            a_val, a_gate, q, k, v = jnp.split(
                proj, [CONV_CH, 2 * CONV_CH, 2 * CONV_CH + DIFF_QK, 2 * CONV_CH + 2 * DIFF_QK], axis=-1)
            y_a = conformer_conv(a_val, a_gate, e_conv_w[i], e_conv_b[i], e_conv_ln_g[i], e_conv_ln_b[i])
            q = rope(q.reshape(bsz, s, 2 * DIFF_HEADS, DIFF_HEAD_DIM), positions).transpose(0, 2, 1, 3)
            k = rope(k.reshape(bsz, s, 2 * DIFF_HEADS, DIFF_HEAD_DIM), positions).transpose(0, 2, 1, 3)
            v = v.reshape(bsz, s, DIFF_HEADS, DIFF_V_DIM).transpose(0, 2, 1, 3)
            lam_p = e_diff_lambda[i].astype(jnp.float32)
            lambda_init = 0.8 - 0.6 * math.exp(-0.3 * layer)
            lam = (jnp.exp(jnp.sum(lam_p[0] * lam_p[1])) - jnp.exp(jnp.sum(lam_p[2] * lam_p[3]))
                   + lambda_init)
            y_b = diff_attention(q, k, v, lam, e_diff_subln[i], lambda_init)
            y = jnp.concatenate([y_a, y_b.astype(y_a.dtype)], axis=-1) @ e_w_out[i]
            x = x + g_m[:, None, :] * y
            h = modulate(rms_norm(x, norm_gains[layer, 1]), sh_f, sc_f)
            x = x + g_f[:, None, :] * swiglu(h, e_ffn_gate[i], e_ffn_up[i], e_ffn_down[i])
        else:
            i = layer // 2
            proj = h @ o_w_in[i]
            q, k, v, r, g1 = jnp.split(
                proj, [GLA_K, 2 * GLA_K, 2 * GLA_K + GLA_V, 2 * GLA_K + 2 * GLA_V], axis=-1)
            gpre = g1 @ o_gate_w2[i] + o_gate_b[i]
            log_a = jax.nn.log_sigmoid(gpre.astype(jnp.float32)) / GLA_TAU
            heads = lambda t, dh: t.reshape(bsz, s, GLA_HEADS, dh).transpose(0, 2, 1, 3)
            o = gla_chunked(heads(q * GLA_DK ** -0.5, GLA_DK), heads(k, GLA_DK),
                            heads(v, GLA_DV), heads(log_a, GLA_DK))
            o = rms_norm(o, o_gla_norm[i]).transpose(0, 2, 1, 3).reshape(bsz, s, GLA_V)
            y = (o.astype(x.dtype) * jax.nn.silu(r)) @ o_w_out[i]
            x = x + g_m[:, None, :] * y
            h = modulate(rms_norm(x, norm_gains[layer, 1]), sh_f, sc_f)
            x = x + g_f[:, None, :] * moe_swiglu(h, o_router[i], o_exp_gate[i], o_exp_up[i], o_exp_down[i])
    return rms_norm(x, final_norm)
```

```python
import numpy as np
import ml_dtypes
import concourse.bass as bass
import concourse.mybir as mybir
from concourse.bass_utils import run_bass_kernel_spmd

F32 = mybir.dt.float32
BF16 = mybir.dt.bfloat16
AF = mybir.ActivationFunctionType
ALU = mybir.AluOpType
AX = mybir.AxisListType
NPBF = ml_dtypes.bfloat16

NSLOT = 8
NC = 8


class T:
    __slots__ = ("w", "r", "rd", "name")

    def __init__(self, name=""):
        self.w = None
        self.r = {}
        self.rd = []
        self.name = name


class Op:
    __slots__ = ("eng", "fn", "idx", "deps", "signal", "dma", "slot", "val", "cnt", "pool", "inc")


class Prog:
    ENGS = ("pe", "act", "dve", "pool", "sp")

    def __init__(self, nc):
        self.nc = nc
        self.ops = {e: [] for e in self.ENGS}
        self.seen = {e: {f: -1 for f in self.ENGS} for e in self.ENGS}
        self.seen_dma = {e: {} for e in self.ENGS}
        self.ndma = {}
        self.slot_last = {}
        self.out_dmas = []
        self.psum = []
        self.psum_i = 0

    def op(self, eng, fn, reads=(), writes=(), dma=False, is_out=False, pool=None, inc=16):
        o = Op()
        o.eng = eng
        o.fn = fn
        o.idx = len(self.ops[eng])
        o.signal = False
        o.dma = dma
        o.cnt = 0
        o.inc = inc
        deps = []
        for t in reads:
            if t.w is not None:
                deps.append((t.w, True))
        for t in writes:
            if t.w is not None:
                deps.append((t.w, False))
            for r in t.r.values():
                deps.append((r, False))
            for r in t.rd:
                deps.append((r, False))
        if dma:
            pool = pool or eng
            o.pool = pool
            k = self.ndma.get(pool, 0)
            self.ndma[pool] = k + 1
            o.slot = k % NSLOT
            o.val = inc * (k // NSLOT + 1)
            sl = self.slot_last.setdefault(pool, [None] * NSLOT)
            prev = sl[o.slot]
            if prev is not None:
                deps.append((prev, True))
            sl[o.slot] = o
        final = []
        seen = self.seen[eng]
        sd = self.seen_dma[eng]
        for d, raw in deps:
            if d.dma:
                key = (d.pool, d.slot)
                if sd.get(key, 0) >= d.val:
                    continue
                sd[key] = d.val
                final.append(d)
            else:
                if d.eng == eng and not raw and not dma:
                    continue
                if seen[d.eng] >= d.idx:
                    continue
                seen[d.eng] = d.idx
                d.signal = True
                final.append(d)
        o.deps = final
        for t in reads:
            if dma:
                t.rd.append(o)
            else:
                t.r[eng] = o
        for t in writes:
            t.w = o
            t.r = {}
            t.rd = []
        self.ops[eng].append(o)
        if is_out:
            self.out_dmas.append(o)
        return o

    def emit(self):
        nc = self.nc
        if self.out_dmas:
            o = Op()
            o.eng = "sp"
            o.fn = None
            o.idx = len(self.ops["sp"])
            o.signal = False
            o.dma = False
            o.cnt = 0
            o.deps = list(self.out_dmas)
            self.ops["sp"].append(o)
        prog_sem = {e: nc.alloc_semaphore("ps_" + e) for e in self.ENGS}
        slot_sem = {p: [nc.alloc_semaphore("ds_%s_%d" % (p, i)) for i in range(NSLOT)]
                    for p in self.ndma}
        for e in self.ENGS:
            c = 0
            for o in self.ops[e]:
                if o.signal and not o.dma:
                    c += 1
                o.cnt = c

        def run(ename, eng):
            for o in self.ops[ename]:
                for d in o.deps:
                    if d.dma:
                        eng.wait_ge(slot_sem[d.pool][d.slot], d.val)
                    else:
                        eng.wait_ge(prog_sem[d.eng], d.cnt)
                if o.fn is None:
                    continue
                ins = o.fn(eng)
                if o.dma:
                    ins.then_inc(slot_sem[o.pool][o.slot], o.inc)
                elif o.signal:
                    ins.then_inc(prog_sem[ename], 1)

        with nc.Block() as block:
            @block.tensor
            def _(e):
                run("pe", e)

            @block.scalar
            def _(e):
                run("act", e)

            @block.vector
            def _(e):
                run("dve", e)

            @block.gpsimd
            def _(e):
                run("pool", e)

            @block.sync
            def _(e):
                run("sp", e)

    def dma(self, eng, out, in_, reads=(), writes=(), is_out=False):
        return self.op(eng, lambda e: e.dma_start(out=out, in_=in_), reads, writes,
                       dma=True, is_out=is_out)

    def cc(self, in_ap, out_ap, groups, reads=(), writes=()):
        return self.op("pool", lambda e: e.collective_compute(
            "AllGather", ALU.bypass, replica_groups=groups, ins=[in_ap], outs=[out_ap]),
            reads, writes, dma=True, pool="cc", inc=1)

    def mm(self, out, lhsT, rhs, start, stop, reads=(), writes=()):
        return self.op("pe", lambda e: e.matmul(out, lhsT, rhs, start=start, stop=stop),
                       reads, writes)

    def act(self, out, in_, func, reads=(), writes=(), **kw):
        return self.op("act", lambda e: e.activation(out=out, in_=in_, func=func, **kw), reads, writes)

    def tt(self, eng, out, in0, in1, op, reads=(), writes=()):
        return self.op(eng, lambda e: e.tensor_tensor(out=out, in0=in0, in1=in1, op=op), reads, writes)

    def ts(self, eng, out, in0, s1, s2, op0, op1=None, reads=(), writes=()):
        if op1 is None:
            return self.op(eng, lambda e: e.tensor_scalar(out=out, in0=in0, scalar1=s1, scalar2=None, op0=op0),
                           reads, writes)
        return self.op(eng, lambda e: e.tensor_scalar(out=out, in0=in0, scalar1=s1, scalar2=s2, op0=op0, op1=op1),
                       reads, writes)

    def stt(self, eng, out, in0, scalar, in1, op0, op1, reads=(), writes=()):
        return self.op(eng, lambda e: e.scalar_tensor_tensor(out=out, in0=in0, scalar=scalar, in1=in1,
                                                             op0=op0, op1=op1), reads, writes)

    def alloc_psum(self, n=8):
        for i in range(n):
            h = self.nc.alloc_psum_tensor("psb%d" % i, [128, 512], F32)
            self.psum.append((h, T("psum%d" % i)))

    def next_psum(self):
        r = self.psum[self.psum_i % len(self.psum)]
        self.psum_i += 1
        return r

    def sb(self, name, shape, dt):
        return self.nc.alloc_sbuf_tensor(name, shape, dt), T(name)


def new_nc():
    return bass.Bass("TRN2", target_bir_lowering=False)


def din(nc, name, shape, dt=F32):
    return nc.dram_tensor(name, list(shape), dt, kind="ExternalInput").ap()


def dout(nc, name, shape, dt=F32):
    return nc.dram_tensor(name, list(shape), dt, kind="ExternalOutput").ap()


def dint(nc, name, shape, dt=F32):
    return nc.dram_tensor(name, list(shape), dt, kind="Internal").ap()


def wchunks(N, rows_per_rank):
    cw_max = max(256, (4 * 1024 * 1024 // 4 // rows_per_rank) // 256 * 256)
    out = []
    c0 = 0
    while c0 < N:
        cw = min(cw_max, N - c0)
        out.append((c0, cw))
        c0 += cw
    return out


def host_wshards(W):
    K, N = W.shape
    rp = K // NC
    ch = wchunks(N, rp)
    res = []
    for r in range(NC):
        blk = W[r * rp:(r + 1) * rp]
        res.append(np.concatenate([np.ascontiguousarray(blk[:, c0:c0 + cw]).ravel() for c0, cw in ch]))
    return res


class GW:
    pass


def ag_weight(P, nc, name, K, N):
    rp = K // NC
    ch = wchunks(N, rp)
    ext = din(nc, name, [rp * N])
    g = GW()
    g.K = K
    g.N = N
    g.chunks = []
    off = 0
    for i, (c0, cw) in enumerate(ch):
        bn = dint(nc, "%s_b%d" % (name, i), [rp, cw])
        gt = dint(nc, "%s_g%d" % (name, i), [K, cw])
        tb = T()
        tg = T()
        src = ext[off:off + rp * cw].rearrange("(r c) -> r c", c=cw)
        P.dma("sp", bn, src, writes=[tb])
        P.cc(bn, gt, [list(range(NC))], reads=[tb], writes=[tg])
        g.chunks.append((c0, cw, gt, tg))
        off += rp * cw
    return g


CB = 256
KG = 32


class WRing:
    def __init__(self, P, nb=3, kg=KG):
        self.P = P
        self.kg = kg
        self.bufs = [P.sb("wring%d" % i, [128, kg, CB], BF16) for i in range(nb)]
        self.i = 0

    def load(self, src_ap, src_t, kcs, cbw):
        b, t = self.bufs[self.i % len(self.bufs)]
        self.i += 1
        self.P.dma("pool", b[:, 0:kcs, 0:cbw], src_ap, reads=[src_t], writes=[t])
        return b, t


def gemm(P, ring, gw, actT, act_t, tok_blocks, epi, col_lo=0, col_hi=None, act_kc0=0):
    K = gw.K
    KC = K // 128
    col_hi = gw.N if col_hi is None else col_hi
    kgs = [(k0, min(ring.kg, KC - k0)) for k0 in range(0, KC, ring.kg)]
    tiles = []
    for (c0, cw, ap, tg) in gw.chunks:
        lo = max(c0, col_lo)
        hi = min(c0 + cw, col_hi)
        c = lo
        while c < hi:
            w = min(CB, hi - c)
            for (k0, kn) in kgs:
                v = ap.rearrange("(kc p) n -> p kc n", p=128)[:, k0:k0 + kn, c - c0:c - c0 + w]
                tiles.append((c, w, k0, kn, v, tg))
            c += w
    NB = len(ring.bufs)
    loaded = []

    def issue(i):
        c, w, k0, kn, v, tg = tiles[i]
        loaded.append(ring.load(v, tg, kn, w))

    for i in range(min(NB - 1, len(tiles))):
        issue(i)
    ti = 0
    ncolblk = len(tiles) // len(kgs)
    for cbi in range(ncolblk):
        c, w = tiles[ti][0], tiles[ti][1]
        nmc = w // 128
        banks = {}
        for gi, (k0, kn) in enumerate(kgs):
            if ti + NB - 1 < len(tiles):
                issue(ti + NB - 1)
            b, t = loaded[ti]
            ti += 1
            for mc in range(nmc):
                for tbi, (t0, tw) in enumerate(tok_blocks):
                    if gi == 0:
                        banks[(mc, tbi)] = P.next_psum()
                    ps, pt = banks[(mc, tbi)]
                    for kc in range(kn):
                        P.mm(ps[:, 0:tw], b[:, kc, mc * 128:(mc + 1) * 128],
                             actT[:, act_kc0 + k0 + kc, t0:t0 + tw],
                             gi == 0 and kc == 0, gi == len(kgs) - 1 and kc == kn - 1,
                             reads=[t, act_t], writes=[pt])
                    if gi == len(kgs) - 1:
                        epi(c + mc * 128, tbi, (t0, tw), ps, pt)


D = 4096
KC = 32
TT = 1024
HALO = 32
TH = TT + HALO
EPS = 1e-6


def norm_mod(P, src, ntok, A, B, a_t, hT, h_t, hoff, ones, ones_t, xt, xt_t, scr):
    sq0, sq0_t, rstd, rstd_t, tmp0, tmp0_t = scr
    sqs = [(sq0, sq0_t), P.sb("nm_sq1", [128, 256], F32)]
    tmps = [(tmp0, tmp0_t), P.sb("nm_tmp1", [128, 256], F32)]
    sv = src.rearrange("(kc p) t -> p kc t", p=128)
    t0 = 0
    while t0 < ntok:
        tw = min(256, ntok - t0)
        P.dma("sp", xt[:, :, 0:tw], sv[:, :, t0:t0 + tw], writes=[xt_t])
        ps, pt = P.next_psum()
        for kc in range(KC):
            sq, sq_t = sqs[kc % 2]
            P.act(sq[:, 0:tw], xt[:, kc, 0:tw], AF.Square, reads=[xt_t], writes=[sq_t])
            P.mm(ps[:, 0:tw], ones[:, :], sq[:, 0:tw], kc == 0, kc == KC - 1, reads=[ones_t, sq_t], writes=[pt])
        P.ts("dve", rstd[:, 0:tw], ps[:, 0:tw], 1.0 / D, EPS, ALU.mult, ALU.add, reads=[pt], writes=[rstd_t])
        P.act(rstd[:, 0:tw], rstd[:, 0:tw], AF.Sqrt, reads=[rstd_t], writes=[rstd_t])
        P.op("dve", lambda e, tw=tw: e.reciprocal(out=rstd[:, 0:tw], in_=rstd[:, 0:tw]), reads=[rstd_t], writes=[rstd_t])
        for kc in range(KC):
            tmp, tmp_t = tmps[kc % 2]
            P.tt("dve", tmp[:, 0:tw], xt[:, kc, 0:tw], rstd[:, 0:tw], ALU.mult, reads=[xt_t, rstd_t], writes=[tmp_t])
            P.act(hT[:, kc, hoff + t0:hoff + t0 + tw], tmp[:, 0:tw], AF.Identity, reads=[tmp_t, a_t], writes=[h_t],
                  scale=A[:, kc:kc + 1], bias=B[:, kc:kc + 1])
        t0 += tw


def build_l0in():
    nc = new_nc()
    P = Prog(nc)
    P.alloc_psum(8)
    xT = din(nc, "xT", [D, TH])
    sc_in = din(nc, "sc", [128, KC])
    sh_in = din(nc, "sh", [128, KC])
    ng_in = din(nc, "ng", [128, KC])
    cw_in = din(nc, "cw", [128, 16 * 31])
    cp_in = din(nc, "cp", [128, 48])
    halo_in = din(nc, "halo", [128, 1])
    cos_in = din(nc, "cos", [128, TT])
    sin_in = din(nc, "sin", [128, TT])
    rot_in = din(nc, "rotm", [128, 128])
    gw = rep_weight(din(nc, "w_in", [D, 10240]), D, 10240)
    yaT = dout(nc, "yaT", [2048, TT], BF16)
    qT = dout(nc, "qT", [2048, TT], BF16)
    kT = dout(nc, "kT", [2048, TT], BF16)
    vT = dout(nc, "vT", [2048, TT], BF16)
    co_d = dint(nc, "co_d", [2048, TT])

    def ld(name, ap, shape):
        b, t = P.sb(name, shape, F32)
        P.dma("sp", b[:, :], ap, writes=[t])
        return b, t

    sc, sc_t = ld("sc_s", sc_in, [128, KC])
    sh, sh_t = ld("sh_s", sh_in, [128, KC])
    ng, ng_t = ld("ng_s", ng_in, [128, KC])
    cw, cw_t = ld("cw_s", cw_in, [128, 16 * 31])
    cp, cp_t = ld("cp_s", cp_in, [128, 48])
    halo, halo_t = ld("halo_s", halo_in, [128, 1])
    cos, cos_t = ld("cos_s", cos_in, [128, TT])
    sin, sin_t = ld("sin_s", sin_in, [128, TT])
    rotm, rot_t = ld("rot_s", rot_in, [128, 128])
    ones, ones_t = P.sb("ones", [128, 128], F32)
    P.op("dve", lambda e: e.memset(ones[:, :], 1.0), writes=[ones_t])
    A, a_t = P.sb("A", [128, KC], F32)
    P.ts("dve", A[:, :], sc[:, :], 1.0, None, ALU.add, reads=[sc_t], writes=[a_t])
    P.tt("dve", A[:, :], A[:, :], ng[:, :], ALU.mult, reads=[a_t, ng_t], writes=[a_t])
    hT, h_t = P.sb("hT", [128, KC, TH], BF16)
    xt, xt_t = P.sb("xt", [128, KC, 256], F32)
    scr = P.sb("sq", [128, 256], F32) + P.sb("rstd", [128, 256], F32) + P.sb("tmpn", [128, 256], F32)
    P.op("act", lambda e: e.activation(out=sh[:, :], in_=sh[:, :], func=AF.Identity), reads=[sh_t, a_t], writes=[a_t, sh_t])
    norm_mod(P, xT, TH, A, sh, a_t, hT, h_t, 0, ones, ones_t, xt, xt_t, scr)

    ring = WRing(P, 3)
    own = [(HALO, 512), (HALO + 512, 512)]
    allb = [(0, HALO), (HALO, 512), (HALO + 512, 512)]
    ob = [P.sb("ob%d" % i, [128, 512], BF16) for i in range(2)]
    obi = [0]
    qf = [P.sb("qf%d" % i, [128, 512], F32) for i in range(2)]
    t1 = [P.sb("t1_%d" % i, [128, 512], F32) for i in range(2)]
    t2 = [P.sb("t2_%d" % i, [128, 512], F32) for i in range(2)]
    qi = [0]

    def epi_qk(col0, tbi, tb, ps, pt):
        t0, tw = tb
        tl = t0 - HALO
        i = qi[0] % 2
        qi[0] += 1
        q, q_t = qf[i]
        a1, a1_t = t1[i]
        a2, a2_t = t2[i]
        P.act(q[:, :], ps[:, :], AF.Copy, reads=[pt], writes=[q_t])
        ps2, pt2 = P.next_psum()
        P.mm(ps2[:, :], rotm[:, :], q[:, :], True, True, reads=[rot_t, q_t], writes=[pt2])
        P.tt("dve", a1[:, :], q[:, :], cos[:, tl:tl + tw], ALU.mult, reads=[q_t, cos_t], writes=[a1_t])
        P.tt("dve", a2[:, :], ps2[:, :], sin[:, tl:tl + tw], ALU.mult, reads=[pt2, sin_t], writes=[a2_t])
        o, o_t = ob[obi[0] % 2]
        obi[0] += 1
        P.tt("pool", o[:, :], a1[:, :], a2[:, :], ALU.add, reads=[a1_t, a2_t], writes=[o_t])
        dst = qT if col0 < 6144 else kT
        r0 = (col0 - 4096) % 2048
        P.dma("sp", dst[r0:r0 + 128, tl:tl + tw], o[:, :], reads=[o_t], is_out=True)

    gemm(P, ring, gw, hT, h_t, own, epi_qk, 4096, 8192)

    def epi_v(col0, tbi, tb, ps, pt):
        t0, tw = tb
        tl = t0 - HALO
        o, o_t = ob[obi[0] % 2]
        obi[0] += 1
        P.act(o[:, :], ps[:, :], AF.Copy, reads=[pt], writes=[o_t])
        r0 = col0 - 8192
        P.dma("sp", vT[r0:r0 + 128, tl:tl + tw], o[:, :], reads=[o_t], is_out=True)

    gemm(P, ring, gw, hT, h_t, own, epi_v, 8192, 10240)

    sg, sg_t = P.sb("sg", [128, TH], F32)
    U = [P.sb("U%d" % i, [128, TH], F32) for i in range(2)]
    CO = [P.sb("CO%d" % i, [128, TT], F32) for i in range(2)]
    sq2, sq2_t = P.sb("sq2", [128, 512], F32)
    S1 = [P.next_psum() for _ in range(2)]
    S2 = [P.next_psum() for _ in range(2)]
    P.psum = P.psum[4:] if False else P.psum
    gem_banks = [b for b in P.psum if b not in S1 and b not in S2]
    save = P.psum
    P.psum = gem_banks
    P.psum_i = 0
    cod_t = [T() for _ in range(16)]
    for cc in range(16):
        u, u_t = U[cc % 2]
        co, co_t = CO[cc % 2]

        def epi_g(col0, tbi, tb, ps, pt):
            t0, tw = tb
            P.act(sg[:, t0:t0 + tw], ps[:, 0:tw], AF.Sigmoid, reads=[pt], writes=[sg_t])

        def epi_a(col0, tbi, tb, ps, pt, u=u, u_t=u_t):
            t0, tw = tb
            P.tt("dve", u[:, t0:t0 + tw], ps[:, 0:tw], sg[:, t0:t0 + tw], ALU.mult, reads=[pt, sg_t], writes=[u_t])

        gemm(P, ring, gw, hT, h_t, allb, epi_g, 2048 + cc * 128, 2048 + (cc + 1) * 128)
        gemm(P, ring, gw, hT, h_t, allb, epi_a, cc * 128, (cc + 1) * 128)
        P.ts("dve", u[:, 0:HALO], u[:, 0:HALO], halo[:, 0:1], None, ALU.mult, reads=[u_t, halo_t], writes=[u_t])
        for hf, eng in ((0, "dve"), (1, "dve")):
            o0 = hf * 512
            P.ts(eng, co[:, o0:o0 + 512], u[:, o0 + 2:o0 + 2 + 512], cw[:, cc * 31:cc * 31 + 1], cp[:, cc:cc + 1],
                 ALU.mult, ALU.add, reads=[u_t, cw_t, cp_t], writes=[co_t])
            for k in range(1, 31):
                P.stt(eng, co[:, o0:o0 + 512], u[:, o0 + k + 2:o0 + k + 2 + 512], cw[:, cc * 31 + k:cc * 31 + k + 1],
                      co[:, o0:o0 + 512], ALU.mult, ALU.add, reads=[u_t, co_t], writes=[co_t])
        for hf in range(2):
            o0 = hf * 512
            P.mm(S1[hf][0][:, :], ones[:, :], co[:, o0:o0 + 512], cc == 0, cc == 15, reads=[ones_t, co_t], writes=[S1[hf][1]])
            P.act(sq2[:, :], co[:, o0:o0 + 512], AF.Square, reads=[co_t], writes=[sq2_t])
            P.mm(S2[hf][0][:, :], ones[:, :], sq2[:, :], cc == 0, cc == 15, reads=[ones_t, sq2_t], writes=[S2[hf][1]])
        P.dma("sp", co_d[cc * 128:(cc + 1) * 128, :], co[:, :], reads=[co_t], writes=[cod_t[cc]])
    P.psum = save
    mu, mu_t = P.sb("mu", [128, TT], F32)
    rs, rs_t = P.sb("rs", [128, TT], F32)
    for hf in range(2):
        o0 = hf * 512
        P.ts("dve", mu[:, o0:o0 + 512], S1[hf][0][:, :], 1.0 / 2048, None, ALU.mult, reads=[S1[hf][1]], writes=[mu_t])
        P.ts("dve", rs[:, o0:o0 + 512], S2[hf][0][:, :], 1.0 / 2048, None, ALU.mult, reads=[S2[hf][1]], writes=[rs_t])
    m2, m2_t = sg, sg_t
    P.tt("dve", m2[:, 0:TT], mu[:, :], mu[:, :], ALU.mult, reads=[mu_t], writes=[m2_t])
    P.tt("dve", rs[:, :], rs[:, :], m2[:, 0:TT], ALU.subtract, reads=[rs_t, m2_t], writes=[rs_t])
    P.ts("dve", rs[:, :], rs[:, :], EPS, None, ALU.add, reads=[rs_t], writes=[rs_t])
    P.act(rs[:, :], rs[:, :], AF.Sqrt, reads=[rs_t], writes=[rs_t])
    P.op("dve", lambda e: e.reciprocal(out=rs[:, :], in_=rs[:, :]), reads=[rs_t], writes=[rs_t])
    co_all_t = T()
    for cc in range(16):
        co, co_t = CO[cc % 2]
        P.dma("sp", co[:, :], co_d[cc * 128:(cc + 1) * 128, :], reads=[cod_t[cc]], writes=[co_t])
        P.tt("dve", co[:, :], co[:, :], mu[:, :], ALU.subtract, reads=[co_t, mu_t], writes=[co_t])
        P.tt("dve", co[:, :], co[:, :], rs[:, :], ALU.mult, reads=[co_t, rs_t], writes=[co_t])
        for hf in range(2):
            o, o_t = ob[obi[0] % 2]
            obi[0] += 1
            P.act(o[:, :], co[:, hf * 512:(hf + 1) * 512], AF.Silu, reads=[co_t, cp_t], writes=[o_t],
                  scale=cp[:, 16 + cc:17 + cc], bias=cp[:, 32 + cc:33 + cc])
            P.dma("sp", yaT[cc * 128:(cc + 1) * 128, hf * 512:(hf + 1) * 512], o[:, :], reads=[o_t], is_out=True)
    P.emit()
    return nc


def build_ada():
    nc = new_nc()
    P = Prog(nc)
    P.alloc_psum(8)
    cT_in = din(nc, "cT", [128, KC * 2])
    ada = din(nc, "ada_s", [12, D, 512])
    adab_in = din(nc, "adab", [2, 12 * 512])
    modp = dout(nc, "modp", [2, 12 * 512])
    cT, c_t = P.sb("cT_s", [128, KC * 2], F32)
    P.dma("sp", cT[:, :], cT_in, writes=[c_t])
    ab, ab_t = P.sb("ab", [2, 12 * 512], F32)
    P.dma("sp", ab[:, :], adab_in, writes=[ab_t])
    P.act(cT[:, :], cT[:, :], AF.Silu, reads=[c_t], writes=[c_t])
    wt = [P.sb("wt%d" % i, [128, KC, 256], F32) for i in range(2)]
    res, res_t = P.sb("res", [2, 12 * 512], F32)
    i = 0
    for g in range(12):
        for half in range(2):
            w, w_t = wt[i % 2]
            P.dma("sp" if i % 2 else "pool", w[:, :, :],
                  ada[g].rearrange("(kc p) n -> p kc n", p=128)[:, :, half * 256:(half + 1) * 256], writes=[w_t])
            i += 1
            ps, pt = P.next_psum()
            for kc in range(KC):
                P.mm(ps[0:2, 0:256], cT[:, kc * 2:kc * 2 + 2], w[:, kc, :], kc == 0, kc == KC - 1,
                     reads=[w_t, c_t], writes=[pt])
            o0 = g * 512 + half * 256
            P.tt("dve", res[:, o0:o0 + 256], ps[0:2, 0:256], ab[:, o0:o0 + 256], ALU.add, reads=[pt, ab_t, res_t], writes=[res_t])
    P.dma("sp", modp, res[:, :], reads=[res_t], is_out=True)
    P.emit()
    return nc


def build_attn():
    S = 4096
    nc = new_nc()
    P = Prog(nc)
    P.alloc_psum(8)
    qT_in = din(nc, "qT", [4 * 128, S], BF16)
    kT_in = din(nc, "kT", [4 * 128, S], BF16)
    v_in = din(nc, "v", [S, 512], BF16)
    lam_in = din(nc, "lam", [1, 512])
    sub_in = din(nc, "subln", [128, 256])
    msk_in = din(nc, "mask", [4, 128, 512])
    yb = dout(nc, "yb", [S, 512])
    scale = 128.0 ** -0.5
    lam_init = 0.8 - 0.6 * 1.0
    qT, q_t = P.sb("qT_s", [128, 4, S], BF16)
    kT, k_t = P.sb("kT_s", [128, 4, S], BF16)
    for h in range(4):
        P.dma("sp", qT[:, h, :], qT_in[h * 128:(h + 1) * 128, :], writes=[q_t])
        P.dma("sp", kT[:, h, :], kT_in[h * 128:(h + 1) * 128, :], writes=[k_t])
    V, v_t = P.sb("V_s", [128, 32, 2, 260], BF16)
    P.op("dve", lambda e: e.memset(V[:, :, :, 256:257], 1.0), writes=[v_t])
    for hv in range(2):
        P.dma("sp", V[:, :, hv, 0:256], v_in.rearrange("(jb p) c -> p jb c", p=128)[:, :, hv * 256:(hv + 1) * 256],
              reads=[v_t], writes=[v_t])
    msk, m_t = P.sb("msk", [128, 4, 512], BF16)
    mskf, mf_t = P.sb("mskf", [128, 4, 512], F32)
    for o in range(4):
        P.dma("sp", mskf[:, o, :], msk_in[o], writes=[mf_t])
    P.op("dve", lambda e: e.tensor_copy(out=msk[:, :, :], in_=mskf[:, :, :]), reads=[mf_t], writes=[m_t])
    sub, sub_t = P.sb("sub", [128, 256], F32)
    P.dma("sp", sub[:, :], sub_in, writes=[sub_t])
    ones, ones_t = P.sb("ones", [128, 128], F32)
    P.op("dve", lambda e: e.memset(ones[:, :], 1.0), writes=[ones_t])
    onesb, onesb_t = P.sb("onesb", [128, 128], BF16)
    P.op("dve", lambda e: e.memset(onesb[:, :], 1.0), writes=[onesb_t])
    lm, lm_t = P.sb("lm", [1, 512], F32)
    P.dma("sp", lm[:, :], lam_in, writes=[lm_t])
    l2, l2_t = P.sb("l2", [1, 8], F32)
    pr, pr_t = P.sb("pr", [1, 256], F32)
    P.tt("dve", pr[:, 0:128], lm[:, 0:128], lm[:, 128:256], ALU.mult, reads=[lm_t], writes=[pr_t])
    P.tt("dve", pr[:, 128:256], lm[:, 256:384], lm[:, 384:512], ALU.mult, reads=[lm_t], writes=[pr_t])
    P.op("dve", lambda e: e.reduce_sum(out=l2[:, 0:1], in_=pr[:, 0:128], axis=AX.X), reads=[pr_t], writes=[l2_t])
    P.op("dve", lambda e: e.reduce_sum(out=l2[:, 1:2], in_=pr[:, 128:256], axis=AX.X), reads=[pr_t], writes=[l2_t])
    P.act(l2[:, 2:4], l2[:, 0:2], AF.Exp, reads=[l2_t], writes=[l2_t])
    P.tt("dve", l2[:, 4:5], l2[:, 2:3], l2[:, 3:4], ALU.subtract, reads=[l2_t], writes=[l2_t])
    P.ts("dve", l2[:, 5:6], l2[:, 4:5], lam_init, -1.0, ALU.add, ALU.mult, reads=[l2_t], writes=[l2_t])
    psl, psl_t = P.next_psum()
    P.mm(psl[:, 0:1], ones[0:1, :], l2[0:1, 5:6], True, True, reads=[ones_t, l2_t], writes=[psl_t])
    nlam, nlam_t = P.sb("nlam", [128, 1], F32)
    P.op("dve", lambda e: e.tensor_copy(out=nlam[:, :], in_=psl[:, 0:1]), reads=[psl_t], writes=[nlam_t])
    negm, negm_t = P.sb("negm", [128, 4], F32)
    mx, mx_t = P.sb("mx", [128, 16], F32)
    sqbs = [P.sb("sqb%d" % i, [128, 512], BF16) for i in range(2)]
    red, red_t = P.sb("red", [128, 8], F32)
    for h in range(4):
        for which, (src, s_t) in enumerate(((qT, q_t), (kT, k_t))):
            for blk in range(8):
                sqb, sqb_t = sqbs[blk % 2]
                P.tt("pool", sqb[:, :], src[:, h, blk * 512:(blk + 1) * 512], src[:, h, blk * 512:(blk + 1) * 512], ALU.mult,
                     reads=[s_t], writes=[sqb_t])
                ps, pt = P.next_psum()
                P.mm(ps[:, :], onesb[:, :], sqb[:, :], True, True, reads=[onesb_t, sqb_t], writes=[pt])
                P.op("dve", lambda e, ps=ps, blk=blk: e.reduce_max(out=red[:, blk:blk + 1], in_=ps[:, :], axis=AX.X),
                     reads=[pt], writes=[red_t])
            P.op("dve", lambda e, h=h, which=which: e.reduce_max(out=mx[:, h * 2 + which:h * 2 + which + 1], in_=red[:, 0:8], axis=AX.X),
                 reads=[red_t], writes=[mx_t])
        P.tt("dve", mx[:, 8 + h:9 + h], mx[:, h * 2:h * 2 + 1], mx[:, h * 2 + 1:h * 2 + 2], ALU.mult, reads=[mx_t], writes=[mx_t])
    P.ts("dve", mx[:, 8:12], mx[:, 8:12], 1.05, None, ALU.mult, reads=[mx_t], writes=[mx_t])
    P.act(mx[:, 12:16], mx[:, 8:12], AF.Sqrt, reads=[mx_t], writes=[mx_t])
    P.ts("dve", negm[:, :], mx[:, 12:16], -scale, None, ALU.mult, reads=[mx_t], writes=[negm_t])

    PT = [P.sb("PT%d" % i, [128, 512], BF16) for i in range(3)]
    pti = 0
    oacc = [P.sb("oacc%d" % i, [128, 4, 260], F32) for i in range(2)]
    on, on_t = P.sb("on", [128, 4, 256], F32)
    tmpo, tmpo_t = P.sb("tmpo", [128, 4, 256], F32)
    rl, rl_t = P.sb("rl", [128, 8], F32)
    ssq, ssq_t = P.sb("ssq", [128, 4], F32)
    junk, junk_t = P.sb("junk", [128, 256], F32)
    acc_banks = [P.next_psum() for _ in range(4)]
    rest = [b for b in P.psum if b not in acc_banks]
    for dh in range(2):
        for ib in range(8):
            for hh in range(2):
                h = dh * 2 + hh
                P.psum = rest
                njb = 4 * (ib + 1)
                for jb in range(njb):
                    ps, pt = P.next_psum()
                    P.mm(ps[:, :], kT[:, h, jb * 128:(jb + 1) * 128], qT[:, h, ib * 512:(ib + 1) * 512], True, True,
                         reads=[k_t, q_t], writes=[pt])
                    p, p_t = PT[pti % 3]
                    pti += 1
                    P.act(p[:, :], ps[:, :], AF.Exp, reads=[pt, negm_t], writes=[p_t], scale=scale, bias=negm[:, h:h + 1])
                    off = jb * 128 - ib * 512
                    if off >= 0:
                        P.tt("dve", p[:, :], p[:, :], msk[:, off // 128, :], ALU.mult, reads=[p_t, m_t], writes=[p_t])
                    for ii in range(4):
                        if off >= 0 and ii * 128 + 127 < off:
                            continue
                        first = jb == 0
                        last = (jb == ib * 4 + ii) if True else False
                        if jb > ib * 4 + ii:
                            continue
                        ab, ab_t = acc_banks[ii]
                        P.mm(ab[:, 0:257], p[:, ii * 128:(ii + 1) * 128], V[:, jb, dh, 0:257], first, last,
                             reads=[p_t, v_t], writes=[ab_t])
                oa, oa_t = oacc[hh]
                for ii in range(4):
                    ab, ab_t = acc_banks[ii]
                    P.act(oa[:, ii, 0:257], ab[:, 0:257], AF.Copy, reads=[ab_t], writes=[oa_t])
            o0, o0_t = oacc[0]
            o1, o1_t = oacc[1]
            for ii in range(4):
                P.op("dve", lambda e, ii=ii: e.reciprocal(out=rl[:, ii:ii + 1], in_=o0[:, ii, 256:257]), reads=[o0_t], writes=[rl_t])
                P.op("dve", lambda e, ii=ii: e.reciprocal(out=rl[:, 4 + ii:5 + ii], in_=o1[:, ii, 256:257]), reads=[o1_t], writes=[rl_t])
            P.ts("dve", rl[:, 4:8], rl[:, 4:8], nlam[:, 0:1], None, ALU.mult, reads=[rl_t, nlam_t], writes=[rl_t])
            for ii in range(4):
                P.ts("dve", on[:, ii, :], o0[:, ii, 0:256], rl[:, ii:ii + 1], None, ALU.mult, reads=[o0_t, rl_t], writes=[on_t])
                P.stt("dve", on[:, ii, :], o1[:, ii, 0:256], rl[:, 4 + ii:5 + ii], on[:, ii, :], ALU.mult, ALU.add,
                      reads=[o1_t, rl_t, on_t], writes=[on_t])
                P.act(junk[:, :], on[:, ii, :], AF.Square, reads=[on_t], writes=[junk_t])
                P.op("dve", lambda e, ii=ii: e.reduce_sum(out=ssq[:, ii:ii + 1], in_=junk[:, :], axis=AX.X), reads=[junk_t], writes=[ssq_t])
            P.ts("dve", ssq[:, :], ssq[:, :], 1.0 / 256, 1e-5, ALU.mult, ALU.add, reads=[ssq_t], writes=[ssq_t])
            P.act(ssq[:, :], ssq[:, :], AF.Sqrt, reads=[ssq_t], writes=[ssq_t])
            P.op("dve", lambda e: e.reciprocal(out=ssq[:, :], in_=ssq[:, :]), reads=[ssq_t], writes=[ssq_t])
            P.ts("dve", ssq[:, :], ssq[:, :], 1.0 - lam_init, None, ALU.mult, reads=[ssq_t], writes=[ssq_t])
            for ii in range(4):
                P.stt("dve", tmpo[:, ii, :], on[:, ii, :], ssq[:, ii:ii + 1], sub[:, :], ALU.mult, ALU.mult,
                      reads=[on_t, ssq_t, sub_t, tmpo_t], writes=[tmpo_t])
                r0 = ib * 512 + ii * 128
                P.dma("sp", yb[r0:r0 + 128, dh * 256:(dh + 1) * 256], tmpo[:, ii, :], reads=[tmpo_t], is_out=True)
    P.emit()
    return nc


def rep_weight(ap, K, N):
    g = GW()
    g.K = K
    g.N = N
    g.chunks = [(0, N, ap, T())]
    return g


TS = 512
NE = 8


def build_outffn(moe, FF):
    nc = new_nc()
    P = Prog(nc)
    P.alloc_psum(8)
    TL = 512
    xT = din(nc, "xT", [D, TT])
    yT = din(nc, "yT", [D, TT], BF16)
    md_in = din(nc, "md", [128, 6 * KC])
    w_out = din(nc, "w_out", [D, D])
    if moe:
        wr_in = din(nc, "wr", [D, NE])
        id_in = din(nc, "ident", [128, 128])
        wg = din(nc, "w_gate", [NE, D, FF])
        wu = din(nc, "w_up", [NE, D, FF])
        wd = din(nc, "w_down", [NE, FF, D])
    else:
        wg = din(nc, "w_gate", [D, FF])
        wu = din(nc, "w_up", [D, FF])
        wd = din(nc, "w_down", [FF, D])
    x2T = dout(nc, "x2T", [D, TT])
    xres = dint(nc, "xres", [D, TT])
    gwo = rep_weight(w_out, D, D)
    md, md_t = P.sb("md_s", [128, 6 * KC], F32)
    P.dma("sp", md[:, :], md_in, writes=[md_t])
    ones, ones_t = P.sb("ones", [128, 128], F32)
    P.op("dve", lambda e: e.memset(ones[:, :], 1.0), writes=[ones_t])
    A, a_t = P.sb("A", [128, KC], F32)
    P.ts("dve", A[:, :], md[:, 2 * KC:3 * KC], 1.0, None, ALU.add, reads=[md_t], writes=[a_t])
    P.tt("dve", A[:, :], A[:, :], md[:, 4 * KC:5 * KC], ALU.mult, reads=[a_t, md_t], writes=[a_t])
    actT, act_t = P.sb("actT", [128, KC, TS], BF16)
    ring = WRing(P, 4, 16)
    xb = [P.sb("xb%d" % i, [128, 512], F32) for i in range(3)]
    xo = [P.sb("xo%d" % i, [128, 512], F32) for i in range(3)]
    cnt = [0]
    sqs = [P.sb("sq%d" % i, [128, TL], F32) for i in range(2)]
    tmps = [P.sb("tmpn%d" % i, [128, 128], F32) for i in range(2)]
    rstd, rstd_t = P.sb("rstd", [128, TL], F32)
    aT, aT_t = P.sb("aT", [128, FF // 128, TL], BF16)
    if moe:
        wr, wr_t = P.sb("wr_s", [128, KC, NE], F32)
        P.dma("sp", wr[:, :, :], wr_in.rearrange("(kc p) e -> p kc e", p=128), writes=[wr_t])
        ident, id_t = P.sb("ident_s", [128, 128], F32)
        P.dma("sp", ident[:, :], id_in, writes=[id_t])
        h32 = [P.sb("h32_%d" % i, [128, 128], F32) for i in range(2)]
        gts, g_t = P.sb("gts", [128, TS // 128, NE], F32)
        lg, lg2, eq1, eq2 = [P.sb(n, [128, NE], F32)[0] for n in ("lg", "lg2", "eq1", "eq2")]
        m1 = P.sb("m1", [128, 8], F32)[0]
        sc_t = T("gscratch")
        bcs = [P.sb("bc%d" % i, [128, 128], F32) for i in range(2)]
        Gs = [P.sb("G%d" % i, [128, TL], F32) for i in range(2)]
        tmpu = [P.sb("tmpu%d" % i, [128, TL], F32) for i in range(2)]
        acc, acc_t = P.sb("acc", [128, KC, TL], F32)
        xt, xt_t = acc, acc_t
    else:
        xt, xt_t = P.sb("xt", [128, KC, 128], F32)
    yv = yT.rearrange("(kc p) t -> p kc t", p=128)
    sv = xres.rearrange("(kc p) t -> p kc t", p=128)
    k2 = [0]
    for s in range(TT // TS):
        s0 = s * TS
        xres_t = [T() for _ in range(KC)]
        for q4 in range(4):
            P.dma("sp", actT[:, q4 * 8:(q4 + 1) * 8, :], yv[:, q4 * 8:(q4 + 1) * 8, s0:s0 + TS], reads=[act_t], writes=[act_t])

        def epi_o(col0, tbi, tb, ps, pt, s0=s0, xres_t=xres_t):
            t0, tw = tb
            c = col0 // 128
            i = cnt[0] % 3
            cnt[0] += 1
            x, x_t = xb[i]
            o, o_t = xo[i]
            P.dma("sp", x[:, 0:tw], xT[col0:col0 + 128, s0 + t0:s0 + t0 + tw], writes=[x_t])
            P.stt("dve", o[:, 0:tw], ps[:, 0:tw], md[:, c:c + 1], x[:, 0:tw], ALU.mult, ALU.add, reads=[pt, md_t, x_t], writes=[o_t])
            P.dma("sp", xres[col0:col0 + 128, s0 + t0:s0 + t0 + tw], o[:, 0:tw], reads=[o_t], writes=[xres_t[c]])

        gemm(P, ring, gwo, actT, act_t, [(0, TS)], epi_o)
        for t0 in range(0, TS, 128):
            tw = 128
            P.dma("sp", xt[:, :, 0:tw], sv[:, :, s0 + t0:s0 + t0 + tw], reads=xres_t, writes=[xt_t])
            ps, pt = P.next_psum()
            for kc in range(KC):
                sq, sq_t = sqs[kc % 2]
                P.act(sq[:, 0:tw], xt[:, kc, 0:tw], AF.Square, reads=[xt_t], writes=[sq_t])
                P.mm(ps[:, 0:tw], ones[:, :], sq[:, 0:tw], kc == 0, kc == KC - 1, reads=[ones_t, sq_t], writes=[pt])
            P.ts("dve", rstd[:, 0:tw], ps[:, 0:tw], 1.0 / D, EPS, ALU.mult, ALU.add, reads=[pt], writes=[rstd_t])
            P.act(rstd[:, 0:tw], rstd[:, 0:tw], AF.Sqrt, reads=[rstd_t], writes=[rstd_t])
            P.op("dve", lambda e, tw=tw: e.reciprocal(out=rstd[:, 0:tw], in_=rstd[:, 0:tw]), reads=[rstd_t], writes=[rstd_t])
            if moe:
                lps, lpt = P.next_psum()
            for kc in range(KC):
                tmp, tmp_t = tmps[kc % 2]
                P.tt("dve", tmp[:, 0:tw], xt[:, kc, 0:tw], rstd[:, 0:tw], ALU.mult, reads=[xt_t, rstd_t], writes=[tmp_t])
                if moe:
                    h, h_t = h32[kc % 2]
                    P.act(h[:, :], tmp[:, 0:tw], AF.Identity, reads=[tmp_t, a_t, md_t], writes=[h_t],
                          scale=A[:, kc:kc + 1], bias=md[:, KC + kc:KC + kc + 1])
                    P.op("pool", lambda e, h=h, kc=kc, t0=t0, tw=tw: e.tensor_copy(out=actT[:, kc, t0:t0 + tw], in_=h[:, :]),
                         reads=[h_t], writes=[act_t])
                    P.mm(lps[:, 0:NE], h[:, :], wr[:, kc, :], kc == 0, kc == KC - 1, reads=[h_t, wr_t], writes=[lpt])
                else:
                    P.act(actT[:, kc, t0:t0 + tw], tmp[:, 0:tw], AF.Identity, reads=[tmp_t, a_t, md_t], writes=[act_t],
                          scale=A[:, kc:kc + 1], bias=md[:, KC + kc:KC + kc + 1])
            if moe:
                tb = t0 // 128
                rw = dict(reads=[sc_t], writes=[sc_t])
                P.op("dve", lambda e, lps=lps: e.tensor_copy(out=lg[:, :], in_=lps[:, 0:NE]), reads=[lpt, sc_t], writes=[sc_t])
                P.op("dve", lambda e: e.reduce_max(out=m1[:, 0:1], in_=lg[:, :], axis=AX.X), **rw)
                P.ts("dve", eq1[:, :], lg[:, :], m1[:, 0:1], None, ALU.is_equal, **rw)
                P.ts("dve", lg2[:, :], eq1[:, :], -1e30, None, ALU.mult, **rw)
                P.tt("dve", lg2[:, :], lg2[:, :], lg[:, :], ALU.add, **rw)
                P.op("dve", lambda e: e.reduce_max(out=m1[:, 1:2], in_=lg2[:, :], axis=AX.X), **rw)
                P.ts("dve", eq2[:, :], lg2[:, :], m1[:, 1:2], None, ALU.is_equal, **rw)
                P.tt("dve", m1[:, 2:3], m1[:, 1:2], m1[:, 0:1], ALU.subtract, **rw)
                P.act(m1[:, 3:4], m1[:, 2:3], AF.Exp, **rw)
                P.ts("dve", m1[:, 4:5], m1[:, 3:4], 1.0, None, ALU.add, **rw)
                P.op("dve", lambda e: e.reciprocal(out=m1[:, 5:6], in_=m1[:, 4:5]), **rw)
                P.tt("dve", m1[:, 6:7], m1[:, 3:4], m1[:, 5:6], ALU.mult, **rw)
                P.ts("dve", gts[:, tb, :], eq1[:, :], m1[:, 5:6], None, ALU.mult, reads=[sc_t, g_t], writes=[g_t])
                P.stt("dve", gts[:, tb, :], eq2[:, :], m1[:, 6:7], gts[:, tb, :], ALU.mult, ALU.add, reads=[sc_t, g_t], writes=[g_t])
        for tl in range(TS // TL):
            tt0 = tl * TL
            g0 = s0 + tt0
            if not moe:
                def epi_g(col0, tbi, tb, ps, pt):
                    P.act(aT[:, col0 // 128, :], ps[:, 0:TL], AF.Silu, reads=[pt], writes=[aT_t])

                def epi_u(col0, tbi, tb, ps, pt):
                    P.tt("dve", aT[:, col0 // 128, :], ps[:, 0:TL], aT[:, col0 // 128, :], ALU.mult, reads=[pt, aT_t], writes=[aT_t])

                def epi_d(col0, tbi, tb, ps, pt, g0=g0, xres_t=xres_t):
                    c = col0 // 128
                    i = cnt[0] % 3
                    cnt[0] += 1
                    x, x_t = xb[i]
                    o, o_t = xo[i]
                    P.dma("sp", x[:, 0:TL], xres[col0:col0 + 128, g0:g0 + TL], reads=[xres_t[c]], writes=[x_t])
                    P.stt("dve", o[:, 0:TL], ps[:, 0:TL], md[:, 3 * KC + c:3 * KC + c + 1], x[:, 0:TL], ALU.mult, ALU.add,
                          reads=[pt, md_t, x_t], writes=[o_t])
                    P.dma("sp", x2T[col0:col0 + 128, g0:g0 + TL], o[:, 0:TL], reads=[o_t], is_out=True)

                gemm(P, ring, rep_weight(wg, D, FF), actT, act_t, [(tt0, TL)], epi_g)
                gemm(P, ring, rep_weight(wu, D, FF), actT, act_t, [(tt0, TL)], epi_u)
                gemm(P, ring, rep_weight(wd, FF, D), aT, aT_t, [(0, TL)], epi_d)
                continue
            for ex in range(NE):
                G, G_t = Gs[ex % 2]
                for bi in range(TL // 128):
                    tb = tt0 // 128 + bi
                    bc, bc_t = bcs[k2[0] % 2]
                    k2[0] += 1
                    P.ts("dve", bc[:, :], ones[:, :], gts[:, tb, ex:ex + 1], None, ALU.mult, reads=[ones_t, g_t], writes=[bc_t])
                    ps, pt = P.next_psum()
                    P.mm(ps[:, 0:128], bc[:, :], ident[:, :], True, True, reads=[bc_t, id_t], writes=[pt])
                    P.act(G[:, bi * 128:(bi + 1) * 128], ps[:, 0:128], AF.Copy, reads=[pt], writes=[G_t])

                def epi_g(col0, tbi, tb, ps, pt):
                    P.act(aT[:, col0 // 128, :], ps[:, 0:TL], AF.Silu, reads=[pt], writes=[aT_t])

                def epi_u(col0, tbi, tb, ps, pt, ex=ex, G=G, G_t=G_t):
                    c = col0 // 128
                    u, u_t = tmpu[k2[0] % 2]
                    k2[0] += 1
                    P.tt("dve", u[:, :], ps[:, 0:TL], G[:, :], ALU.mult, reads=[pt, G_t], writes=[u_t])
                    P.tt("pool", aT[:, c, :], u[:, :], aT[:, c, :], ALU.mult, reads=[u_t, aT_t], writes=[aT_t])

                def epi_d(col0, tbi, tb, ps, pt, ex=ex):
                    c = col0 // 128
                    if ex == 0:
                        P.act(acc[:, c, :], ps[:, 0:TL], AF.Copy, reads=[pt], writes=[acc_t])
                    else:
                        P.tt("dve", acc[:, c, :], acc[:, c, :], ps[:, 0:TL], ALU.add, reads=[pt, acc_t], writes=[acc_t])

                gemm(P, ring, rep_weight(wg[ex], D, FF), actT, act_t, [(tt0, TL)], epi_g)
                gemm(P, ring, rep_weight(wu[ex], D, FF), actT, act_t, [(tt0, TL)], epi_u)
                gemm(P, ring, rep_weight(wd[ex], FF, D), aT, aT_t, [(0, TL)], epi_d)
            fps, fpt = P.next_psum()
            for c in range(KC):
                i = cnt[0] % 3
                cnt[0] += 1
                x, x_t = xb[i]
                P.dma("sp", x[:, 0:TL], xres[c * 128:(c + 1) * 128, g0:g0 + TL], reads=[xres_t[c]], writes=[x_t])
                P.stt("dve", acc[:, c, :], acc[:, c, :], md[:, 3 * KC + c:3 * KC + c + 1], x[:, 0:TL], ALU.mult, ALU.add,
                      reads=[acc_t, md_t, x_t], writes=[acc_t])
                sq, sq_t = sqs[c % 2]
                P.act(sq[:, 0:TL], acc[:, c, :], AF.Square, reads=[acc_t], writes=[sq_t])
                P.mm(fps[:, 0:TL], ones[:, :], sq[:, 0:TL], c == 0, c == KC - 1, reads=[ones_t, sq_t], writes=[fpt])
            P.ts("dve", rstd[:, 0:TL], fps[:, 0:TL], 1.0 / D, EPS, ALU.mult, ALU.add, reads=[fpt], writes=[rstd_t])
            P.act(rstd[:, 0:TL], rstd[:, 0:TL], AF.Sqrt, reads=[rstd_t], writes=[rstd_t])
            P.op("dve", lambda e: e.reciprocal(out=rstd[:, 0:TL], in_=rstd[:, 0:TL]), reads=[rstd_t], writes=[rstd_t])
            for c in range(KC):
                i = cnt[0] % 3
                cnt[0] += 1
                o, o_t = xo[i]
                P.stt("dve", o[:, 0:TL], acc[:, c, :], md[:, 5 * KC + c:5 * KC + c + 1], rstd[:, 0:TL], ALU.mult, ALU.mult,
                      reads=[acc_t, md_t, rstd_t], writes=[o_t])
                P.dma("sp", x2T[c * 128:(c + 1) * 128, g0:g0 + TL], o[:, 0:TL], reads=[o_t], is_out=True)
    P.emit()
    return nc


def build_l1in():
    NIN = 12304
    nc = new_nc()
    P = Prog(nc)
    P.alloc_psum(8)
    xT = din(nc, "xT", [D, TT])
    sc_in = din(nc, "sc", [128, KC])
    sh_in = din(nc, "sh", [128, KC])
    ng_in = din(nc, "ng", [128, KC])
    w_in = din(nc, "w_in", [D, NIN])
    w2_in = din(nc, "w2", [16, 2048])
    gb_in = din(nc, "gb", [128, 2048])
    qT = dout(nc, "qT", [2048, TT], BF16)
    kT = dout(nc, "kT", [2048, TT], BF16)
    vT = dout(nc, "vT", [4096, TT], BF16)
    rT = dout(nc, "rT", [4096, TT], BF16)
    la = dout(nc, "la", [TT, 2048])

    def ld(name, ap, shape):
        b, t = P.sb(name, shape, F32)
        P.dma("sp", b[:, :], ap, writes=[t])
        return b, t

    sc, sc_t = ld("sc_s", sc_in, [128, KC])
    sh, sh_t = ld("sh_s", sh_in, [128, KC])
    ng, ng_t = ld("ng_s", ng_in, [128, KC])
    w2, w2_t = ld("w2_s", w2_in, [16, 2048])
    gb, gb_t = ld("gb_s", gb_in, [128, 2048])
    ones, ones_t = P.sb("ones", [128, 128], F32)
    P.op("dve", lambda e: e.memset(ones[:, :], 1.0), writes=[ones_t])
    A, a_t = P.sb("A", [128, KC], F32)
    P.ts("dve", A[:, :], sc[:, :], 1.0, None, ALU.add, reads=[sc_t], writes=[a_t])
    P.tt("dve", A[:, :], A[:, :], ng[:, :], ALU.mult, reads=[a_t, ng_t], writes=[a_t])
    hT, h_t = P.sb("hT", [128, KC, TT], BF16)
    xt, xt_t = P.sb("xt", [128, KC, 256], F32)
    scr = P.sb("sq", [128, 256], F32) + P.sb("rstd", [128, 256], F32) + P.sb("tmpn", [128, 256], F32)
    P.op("act", lambda e: e.activation(out=sh[:, :], in_=sh[:, :], func=AF.Identity), reads=[sh_t, a_t], writes=[a_t, sh_t])
    norm_mod(P, xT, TT, A, sh, a_t, hT, h_t, 0, ones, ones_t, xt, xt_t, scr)
    ring = WRing(P, 3)
    gw = rep_weight(w_in, D, NIN)
    ob = [P.sb("ob%d" % i, [128, 512], BF16) for i in range(4)]
    obi = [0]
    qscale = 512.0 ** -0.5

    def epi(col0, tbi, tb, ps, pt):
        t0, tw = tb
        o, o_t = ob[obi[0] % 4]
        if col0 < 2048:
            dst, r0 = qT, col0
            P.ts("dve", o[:, 0:tw], ps[:, 0:tw], qscale, None, ALU.mult, reads=[pt], writes=[o_t])
        else:
            if col0 < 4096:
                dst, r0 = kT, col0 - 2048
            elif col0 < 8192:
                dst, r0 = vT, col0 - 4096
            else:
                dst, r0 = rT, col0 - 8192
            if obi[0] % 2 == 0:
                P.act(o[:, 0:tw], ps[:, 0:tw], AF.Copy, reads=[pt], writes=[o_t])
            else:
                P.op("dve", lambda e, o=o, ps=ps, tw=tw: e.tensor_copy(out=o[:, 0:tw], in_=ps[:, 0:tw]), reads=[pt], writes=[o_t])
        obi[0] += 1
        P.dma("sp", dst[r0:r0 + 128, t0:t0 + tw], o[:, 0:tw], reads=[o_t], is_out=True)

    gemm(P, ring, gw, hT, h_t, [(0, 512), (512, 512)], epi, 0, 12288)
    wg1, wg1_t = P.sb("wg1", [128, KC, 16], BF16)
    P.dma("pool", wg1[:, :, :], w_in.rearrange("(kc p) n -> p kc n", p=128)[:, :, 12288:12304], writes=[wg1_t])
    g1T, g1_t = P.sb("g1T", [16, TT], F32)
    for tb in range(2):
        ps, pt = P.next_psum()
        for kc in range(KC):
            P.mm(ps[0:16, :], wg1[:, kc, :], hT[:, kc, tb * 512:(tb + 1) * 512], kc == 0, kc == KC - 1,
                 reads=[wg1_t, h_t], writes=[pt])
        P.act(g1T[:, tb * 512:(tb + 1) * 512], ps[0:16, :], AF.Copy, reads=[pt, g1_t], writes=[g1_t])
    zt = [P.sb("zt%d" % i, [128, 512], F32) for i in range(2)]
    lo = [P.sb("lo%d" % i, [128, 512], F32) for i in range(2)]
    k = 0
    for tb in range(TT // 128):
        for cb in range(4):
            z, z_t = zt[k % 2]
            l, l_t = lo[k % 2]
            k += 1
            ps, pt = P.next_psum()
            P.mm(ps[:, :], g1T[:, tb * 128:(tb + 1) * 128], w2[:, cb * 512:(cb + 1) * 512], True, True,
                 reads=[g1_t, w2_t], writes=[pt])
            P.tt("dve", z[:, :], ps[:, :], gb[:, cb * 512:(cb + 1) * 512], ALU.add, reads=[pt, gb_t], writes=[z_t])
            P.act(z[:, :], z[:, :], AF.Exp, reads=[z_t], writes=[z_t], scale=-1.0)
            P.act(z[:, :], z[:, :], AF.Ln, reads=[z_t, ones_t], writes=[z_t], bias=ones[:, 0:1])
            P.ts("dve", l[:, :], z[:, :], -1.0 / 16.0, None, ALU.mult, reads=[z_t], writes=[l_t])
            P.dma("sp", la[tb * 128:(tb + 1) * 128, cb * 512:(cb + 1) * 512], l[:, :], reads=[l_t], is_out=True)
    P.emit()
    return nc


def build_gla():
    S = 4096
    DK = 512
    DV = 1024
    nc = new_nc()
    P = Prog(nc)
    P.alloc_psum(8)
    qT_in = din(nc, "qT", [DK, S], BF16)
    kT_in = din(nc, "kT", [DK, S], BF16)
    la_in = din(nc, "la", [S, DK])
    v_in = din(nc, "v", [S, DV], BF16)
    r_in = din(nc, "r", [S, DV], BF16)
    gn_in = din(nc, "gn", [128, DV])
    lt_in = din(nc, "lt2", [128, 128])
    mk_in = din(nc, "maskT", [64, 64])
    idb_in = din(nc, "identb", [128, 128], BF16)
    og = dout(nc, "og", [S, DV], BF16)
    qT, q_t = P.sb("qT_s", [128, 4, S], BF16)
    kT, k_t = P.sb("kT_s", [128, 4, S], BF16)
    for dc in range(4):
        P.dma("sp", qT[:, dc, :], qT_in[dc * 128:(dc + 1) * 128, :], reads=[q_t], writes=[q_t])
        P.dma("sp", kT[:, dc, :], kT_in[dc * 128:(dc + 1) * 128, :], reads=[k_t], writes=[k_t])
    gn, gn_t = P.sb("gn_s", [128, DV], F32)
    P.dma("sp", gn[:, :], gn_in, writes=[gn_t])
    lt2, lt_t = P.sb("lt2_s", [128, 128], F32)
    P.dma("sp", lt2[:, :], lt_in, writes=[lt_t])
    mk, mk_t = P.sb("mk_s", [64, 64], F32)
    P.dma("sp", mk[:, :], mk_in, writes=[mk_t])
    idb, idb_t = P.sb("idb_s", [128, 128], BF16)
    P.dma("sp", idb[:, :], idb_in, writes=[idb_t])
    S32, S_t = P.sb("S32", [128, 4, DV], F32)
    Sbf, Sbf_t = P.sb("Sbf", [128, 4, DV], BF16)
    S_ts = [[T() for _ in range(2)] for _ in range(4)]
    Sbf_ts = [[T() for _ in range(2)] for _ in range(4)]
    P.op("dve", lambda e: e.memset(S32[:, :, :], 0.0), writes=[S_t] + [t for r in S_ts for t in r])
    P.op("dve", lambda e: e.memset(Sbf[:, :, :], 0.0), writes=[Sbf_t] + [t for r in Sbf_ts for t in r])
    la_b = [P.sb("la%d" % i, [128, DK], F32) for i in range(2)]
    ebs = [P.sb("eb%d" % i, [128, 4, 128], F32) for i in range(2)]
    enbs = [P.sb("enb%d" % i, [128, 4, 128], F32) for i in range(2)]
    Qts = [P.sb("Qt%d" % i, [128, 4, 128], BF16) for i in range(2)]
    Kts = [P.sb("Kt%d" % i, [128, 4, 128], BF16) for i in range(2)]
    KdTs = [P.sb("KdT%d" % i, [128, 4, 64], BF16) for i in range(2)]
    Kds = [P.sb("Kd%d" % i, [64, DK], BF16) for i in range(2)]
    ats = [P.sb("at%d" % i, [64, 64], BF16) for i in range(2)]
    Vcs = [P.sb("Vc%d" % i, [64, DV], BF16) for i in range(3)]
    Rcs = [P.sb("Rc%d" % i, [64, DV], BF16) for i in range(2)]
    osbs = [P.sb("osb%d" % i, [64, DV], F32) for i in range(2)]
    junk, junk_t = P.sb("junk", [64, DV], F32)
    srs = [P.sb("sr%d" % i, [64, DV], F32) for i in range(2)]
    ons = [P.sb("on%d" % i, [64, DV], F32) for i in range(2)]
    ogts = [P.sb("ogt%d" % i, [64, DV], BF16) for i in range(2)]
    ssqs = [P.sb("ssq%d" % i, [64, 2], F32) for i in range(2)]
    ci = 0
    for pb in range(S // 128):
        t0 = pb * 128
        la, la_t = la_b[pb % 2]
        eb, eb_t = ebs[pb % 2]
        enb, enb_t = enbs[pb % 2]
        Qt, Qt_t = Qts[pb % 2]
        Kt, Kt_t = Kts[pb % 2]
        P.dma("sp", la[:, :], la_in[t0:t0 + 128, :], writes=[la_t])
        bps, bpt = P.next_psum()
        for dc in range(4):
            P.mm(bps[:, dc * 128:(dc + 1) * 128], la[:, dc * 128:(dc + 1) * 128], lt2[:, :], True, True,
                 reads=[la_t, lt_t], writes=[bpt])
        for dc in range(4):
            P.act(eb[:, dc, :], bps[:, dc * 128:(dc + 1) * 128], AF.Exp, reads=[bpt], writes=[eb_t])
            P.act(enb[:, dc, :], bps[:, dc * 128:(dc + 1) * 128], AF.Exp, reads=[bpt], writes=[enb_t], scale=-1.0)
            P.tt("dve", Qt[:, dc, :], qT[:, dc, t0:t0 + 128], eb[:, dc, :], ALU.mult, reads=[q_t, eb_t], writes=[Qt_t])
            P.tt("pool", Kt[:, dc, :], kT[:, dc, t0:t0 + 128], enb[:, dc, :], ALU.mult, reads=[k_t, enb_t], writes=[Kt_t])
        for c in range(2):
            tc = t0 + c * 64
            c0, c1 = c * 64, (c + 1) * 64
            Vc, Vc_t = Vcs[ci % 3]
            Rc, Rc_t = Rcs[ci % 2]
            KdT, KdT_t = KdTs[ci % 2]
            Kd, Kd_t = Kds[ci % 2]
            at, at_t = ats[ci % 2]
            osb, o_t = osbs[ci % 2]
            sr, sr_t = srs[ci % 2]
            on, on_t = ons[ci % 2]
            ogt, ogt_t = ogts[ci % 2]
            ssq, ssq_t = ssqs[ci % 2]
            ci += 1
            P.dma("sp", Vc[:, :], v_in[tc:tc + 64, :], writes=[Vc_t])
            P.dma("sp", Rc[:, :], r_in[tc:tc + 64, :], writes=[Rc_t])
            for dc in range(4):
                P.ts("dve", KdT[:, dc, :], Kt[:, dc, c0:c1], eb[:, dc, c1 - 1:c1], None, ALU.mult,
                     reads=[Kt_t, eb_t], writes=[KdT_t])
            aps, apt = P.next_psum()
            for dc in range(4):
                P.mm(aps[0:64, 0:64], Kt[:, dc, c0:c1], Qt[:, dc, c0:c1], dc == 0, dc == 3, reads=[Kt_t, Qt_t], writes=[apt])
            P.tt("dve", at[:, :], aps[0:64, 0:64], mk[:, :], ALU.mult, reads=[apt, mk_t], writes=[at_t])
            kps, kpt = P.next_psum()
            for dc in range(4):
                P.mm(kps[0:64, dc * 128:(dc + 1) * 128], KdT[:, dc, :], idb[:, :], True, True, reads=[KdT_t, idb_t], writes=[kpt])
            P.act(Kd[:, :], kps[0:64, :], AF.Copy, reads=[kpt], writes=[Kd_t])
            obanks = [P.next_psum() for _ in range(2)]
            for hf in range(2):
                ops, opt = obanks[hf]
                P.mm(ops[0:64, :], at[:, :], Vc[:, hf * 512:(hf + 1) * 512], True, False, reads=[at_t, Vc_t], writes=[opt])
                for dc in range(4):
                    P.mm(ops[0:64, :], Qt[:, dc, c0:c1], Sbf[:, dc, hf * 512:(hf + 1) * 512], False, dc == 3,
                         reads=[Qt_t, Sbf_ts[dc][hf]], writes=[opt])
            for hf in range(2):
                ops, opt = obanks[hf]
                P.act(osb[:, hf * 512:(hf + 1) * 512], ops[0:64, :], AF.Copy, reads=[opt], writes=[o_t])
            for dc in range(4):
                for hf in range(2):
                    sps, spt = P.next_psum()
                    P.mm(sps[:, :], Kd[:, dc * 128:(dc + 1) * 128], Vc[:, hf * 512:(hf + 1) * 512], True, True,
                         reads=[Kd_t, Vc_t], writes=[spt])
                    P.stt("dve", S32[:, dc, hf * 512:(hf + 1) * 512], S32[:, dc, hf * 512:(hf + 1) * 512], eb[:, dc, c1 - 1:c1],
                          sps[:, :], ALU.mult, ALU.add, reads=[S_ts[dc][hf], eb_t, spt], writes=[S_ts[dc][hf]])
                    P.op("pool", lambda e, dc=dc, hf=hf: e.tensor_copy(out=Sbf[:, dc, hf * 512:(hf + 1) * 512],
                                                                        in_=S32[:, dc, hf * 512:(hf + 1) * 512]),
                         reads=[S_ts[dc][hf]], writes=[Sbf_ts[dc][hf]])
            P.act(junk[:, :], osb[:, :], AF.Square, reads=[o_t], writes=[junk_t])
            P.op("dve", lambda e, ssq=ssq: e.reduce_sum(out=ssq[:, 0:1], in_=junk[:, :], axis=AX.X), reads=[junk_t], writes=[ssq_t])
            P.ts("dve", ssq[:, 0:1], ssq[:, 0:1], 1.0 / DV, EPS, ALU.mult, ALU.add, reads=[ssq_t], writes=[ssq_t])
            P.act(ssq[:, 0:1], ssq[:, 0:1], AF.Sqrt, reads=[ssq_t], writes=[ssq_t])
            P.op("dve", lambda e, ssq=ssq: e.reciprocal(out=ssq[:, 0:1], in_=ssq[:, 0:1]), reads=[ssq_t], writes=[ssq_t])
            P.act(sr[:, :], Rc[:, :], AF.Silu, reads=[Rc_t], writes=[sr_t])
            P.stt("dve", on[:, :], osb[:, :], ssq[:, 0:1], gn[0:64, :], ALU.mult, ALU.mult, reads=[o_t, ssq_t, gn_t], writes=[on_t])
            P.tt("pool", ogt[:, :], on[:, :], sr[:, :], ALU.mult, reads=[on_t, sr_t], writes=[ogt_t])
            P.dma("pool", og[tc:tc + 64, :], ogt[:, :], reads=[ogt_t], is_out=True)
    P.emit()
    return nc


def _fm(v):
    return np.ascontiguousarray(np.asarray(v, np.float32).reshape(32, 128).T)


def _rope_tables(t0):
    d = 128
    inv = 10000.0 ** (-np.arange(0, d, 2, dtype=np.float32) / d)
    pos = np.arange(t0, t0 + TT, dtype=np.float32)
    ang = pos[:, None] * inv[None, :]
    cos = np.cos(ang).astype(np.float32).T
    sin = np.sin(ang).astype(np.float32).T
    return np.ascontiguousarray(np.concatenate([cos, cos], 0)), np.ascontiguousarray(np.concatenate([sin, sin], 0))


def _rotm():
    R = np.zeros((128, 128), np.float32)
    for d in range(64):
        R[d + 64, d] = -1.0
        R[d, d + 64] = 1.0
    return R


def _diag_masks():
    m = np.zeros((4, 128, 512), np.float32)
    j = np.arange(128)[:, None]
    i = np.arange(512)[None, :]
    for o in range(4):
        m[o] = (i >= j + o * 128).astype(np.float32)
    return m


def _gla_consts():
    j = np.arange(128)[:, None]
    i = np.arange(128)[None, :]
    lt2 = ((j <= i) & (j // 64 == i // 64)).astype(np.float32)
    mk = (np.arange(64)[:, None] <= np.arange(64)[None, :]).astype(np.float32)
    return np.ascontiguousarray(lt2), np.ascontiguousarray(mk), np.eye(128, dtype=np.float32).astype(NPBF)


def _run(nc, in_maps):
    return run_bass_kernel_spmd(nc, in_maps, core_ids=list(range(NC))).results


def _c(a):
    return np.ascontiguousarray(np.asarray(a, np.float32))


def kernel(x, c, norm_gains, ada_w, ada_b, e_w_in, e_conv_w, e_conv_b, e_conv_ln_g, e_conv_ln_b,
           e_diff_lambda, e_diff_subln, e_w_out, e_ffn_gate, e_ffn_up, e_ffn_down, o_w_in, o_gate_w2,
           o_gate_b, o_gla_norm, o_w_out, o_router, o_exp_gate, o_exp_up, o_exp_down, final_norm):
    x = np.asarray(x, np.float32)
    B, S, _ = x.shape
    cT = np.ascontiguousarray(np.asarray(c).T.reshape(32, 128, 2).transpose(1, 0, 2).reshape(128, 64))
    ims = []
    for r in range(NC):
        sl = np.stack([np.asarray(ada_w[l])[:, w * D + r * 512:w * D + (r + 1) * 512] for l in range(2) for w in range(6)])
        ab = np.concatenate([np.asarray(ada_b[l], np.float32)[w * D + r * 512:w * D + (r + 1) * 512] for l in range(2) for w in range(6)])
        ims.append({"cT": cT, "ada_s": np.ascontiguousarray(sl), "adab": np.ascontiguousarray(np.tile(ab[None], (2, 1)))})
    res = _run(build_ada(), ims)
    del ims
    modv = np.concatenate([res[r]["modp"].reshape(2, 12, 512) for r in range(NC)], axis=2)

    def mod(l, w, b):
        return _fm(modv[b, l * 6 + w])

    fin = _fm(final_norm)
    w0 = _c(e_w_in[0])
    cwT = np.ascontiguousarray(np.asarray(e_conv_w[0]).T.reshape(16, 128, 31).transpose(1, 0, 2).reshape(128, 16 * 31))
    cp = np.ascontiguousarray(np.concatenate([np.asarray(e_conv_b[0]).reshape(16, 128).T,
                                              np.asarray(e_conv_ln_g[0]).reshape(16, 128).T,
                                              np.asarray(e_conv_ln_b[0]).reshape(16, 128).T], axis=1).astype(np.float32))
    rotm = _rotm()
    ims = []
    for r in range(NC):
        b, j = r // 4, r % 4
        t0 = j * TT
        xs = np.zeros((TH, D), np.float32)
        if j > 0:
            xs[:] = x[b, t0 - HALO:t0 + TT]
        else:
            xs[HALO:] = x[b, 0:TT]
        cs, sn = _rope_tables(t0)
        ims.append({"xT": np.ascontiguousarray(xs.T), "sc": mod(0, 1, b), "sh": mod(0, 0, b), "ng": _fm(norm_gains[0, 0]),
                    "cw": cwT, "cp": cp, "halo": np.full((128, 1), 0.0 if j == 0 else 1.0, np.float32),
                    "cos": cs, "sin": sn, "rotm": rotm, "w_in": w0})
    res = _run(build_l0in(), ims)
    del ims, w0
    qT = [np.concatenate([res[b * 4 + j]["qT"] for j in range(4)], axis=1) for b in range(B)]
    kT = [np.concatenate([res[b * 4 + j]["kT"] for j in range(4)], axis=1) for b in range(B)]
    vT = [np.concatenate([res[b * 4 + j]["vT"] for j in range(4)], axis=1) for b in range(B)]
    yaT = [res[r]["yaT"] for r in range(NC)]
    lam = np.ascontiguousarray(np.asarray(e_diff_lambda[0], np.float32).reshape(1, 512))
    sub = np.ascontiguousarray(np.tile(np.asarray(e_diff_subln[0], np.float32)[None], (128, 1)))
    msk = _diag_masks()
    ims = []
    for r in range(NC):
        b, hp = r // 4, r % 4
        ims.append({"qT": np.ascontiguousarray(qT[b][hp * 512:(hp + 1) * 512]),
                    "kT": np.ascontiguousarray(kT[b][hp * 512:(hp + 1) * 512]),
                    "v": np.ascontiguousarray(vT[b][hp * 512:(hp + 1) * 512].T),
                    "lam": lam, "subln": sub, "mask": msk})
    res = _run(build_attn(), ims)
    del ims
    yb = [np.concatenate([res[b * 4 + hp]["yb"] for hp in range(4)], axis=1) for b in range(B)]
    wo, wg, wu, wd = _c(e_w_out[0]), _c(e_ffn_gate[0]), _c(e_ffn_up[0]), _c(e_ffn_down[0])
    ims = []
    for r in range(NC):
        b, j = r // 4, r % 4
        t0 = j * TT
        ybT = np.ascontiguousarray(yb[b][t0:t0 + TT].T).astype(NPBF)
        md = np.concatenate([mod(0, 2, b), mod(0, 3, b), mod(0, 4, b), mod(0, 5, b), _fm(norm_gains[0, 1]), fin], axis=1)
        ims.append({"xT": np.ascontiguousarray(x[b, t0:t0 + TT].T), "yT": np.ascontiguousarray(np.concatenate([yaT[r], ybT], axis=0)),
                    "md": np.ascontiguousarray(md), "w_out": wo, "w_gate": wg, "w_up": wu, "w_down": wd})
    res = _run(build_outffn(False, 11008), ims)
    del ims, wo, wg, wu, wd
    x1T = [res[r]["x2T"] for r in range(NC)]
    w1 = _c(o_w_in[0])
    w2 = _c(o_gate_w2[0])
    gb = np.ascontiguousarray(np.tile(np.asarray(o_gate_b[0], np.float32)[None], (128, 1)))
    ims = []
    for r in range(NC):
        b = r // 4
        ims.append({"xT": x1T[r], "sc": mod(1, 1, b), "sh": mod(1, 0, b), "ng": _fm(norm_gains[1, 0]),
                    "w_in": w1, "w2": w2, "gb": gb})
    res = _run(build_l1in(), ims)
    del ims, w1
    lt2, mk, idb = _gla_consts()
    gn = np.ascontiguousarray(np.tile(np.asarray(o_gla_norm[0], np.float32)[None], (128, 1)))
    cat = lambda name, b, ax: np.concatenate([res[b * 4 + j][name] for j in range(4)], axis=ax)
    ims = []
    for b in range(B):
        qb, kb, vb, rb, lab = cat("qT", b, 1), cat("kT", b, 1), cat("vT", b, 1), cat("rT", b, 1), cat("la", b, 0)
        for h in range(4):
            ims.append({"qT": np.ascontiguousarray(qb[h * 512:(h + 1) * 512]), "kT": np.ascontiguousarray(kb[h * 512:(h + 1) * 512]),
                        "la": np.ascontiguousarray(lab[:, h * 512:(h + 1) * 512]),
                        "v": np.ascontiguousarray(vb[h * 1024:(h + 1) * 1024].T), "r": np.ascontiguousarray(rb[h * 1024:(h + 1) * 1024].T),
                        "gn": gn, "lt2": lt2, "maskT": mk, "identb": idb})
    res = _run(build_gla(), ims)
    del ims
    wo, wr = _c(o_w_out[0]), _c(o_router[0])
    eg, eu, ed = _c(o_exp_gate[0]), _c(o_exp_up[0]), _c(o_exp_down[0])
    ident = np.eye(128, dtype=np.float32)
    ims = []
    for r in range(NC):
        b, j = r // 4, r % 4
        yT = np.ascontiguousarray(np.concatenate([res[b * 4 + h]["og"][j * TT:(j + 1) * TT].T for h in range(4)], axis=0))
        md = np.concatenate([mod(1, 2, b), mod(1, 3, b), mod(1, 4, b), mod(1, 5, b), _fm(norm_gains[1, 1]), fin], axis=1)
        ims.append({"xT": x1T[r], "yT": yT, "md": np.ascontiguousarray(md), "w_out": wo, "wr": wr, "ident": ident,
                    "w_gate": eg, "w_up": eu, "w_down": ed})
    res = _run(build_outffn(True, 4096), ims)
    out = np.zeros((B, S, D), np.float32)
    for r in range(NC):
        b, j = r // 4, r % 4
        out[b, j * TT:(j + 1) * TT] = res[r]["x2T"].T
    return out
```

```python
import numpy as np
import ml_dtypes
import concourse.bass as bass
import concourse.mybir as mybir
from concourse.bass_utils import run_bass_kernel_spmd

F32 = mybir.dt.float32
BF16 = mybir.dt.bfloat16
AF = mybir.ActivationFunctionType
ALU = mybir.AluOpType
AX = mybir.AxisListType
NPBF = ml_dtypes.bfloat16

NSLOT = 8
NC = 8


class T:
    __slots__ = ("w", "r", "rd", "name")

    def __init__(self, name=""):
        self.w = None
        self.r = {}
        self.rd = []
        self.name = name


class Op:
    __slots__ = ("eng", "fn", "idx", "deps", "signal", "dma", "slot", "val", "cnt", "pool", "inc")


class Prog:
    ENGS = ("pe", "act", "dve", "pool", "sp")

    def __init__(self, nc):
        self.nc = nc
        self.ops = {e: [] for e in self.ENGS}
        self.seen = {e: {f: -1 for f in self.ENGS} for e in self.ENGS}
        self.seen_dma = {e: {} for e in self.ENGS}
        self.ndma = {}
        self.slot_last = {}
        self.out_dmas = []
        self.psum = []
        self.psum_i = 0

    def op(self, eng, fn, reads=(), writes=(), dma=False, is_out=False, pool=None, inc=16):
        o = Op()
        o.eng = eng
        o.fn = fn
        o.idx = len(self.ops[eng])
        o.signal = False
        o.dma = dma
        o.cnt = 0
        o.inc = inc
        deps = []
        for t in reads:
            if t.w is not None:
                deps.append((t.w, True))
        for t in writes:
            if t.w is not None:
                deps.append((t.w, False))
            for r in t.r.values():
                deps.append((r, False))
            for r in t.rd:
                deps.append((r, False))
        if dma:
            pool = pool or eng
            o.pool = pool
            k = self.ndma.get(pool, 0)
            self.ndma[pool] = k + 1
            o.slot = k % NSLOT
            o.val = inc * (k // NSLOT + 1)
            sl = self.slot_last.setdefault(pool, [None] * NSLOT)
            prev = sl[o.slot]
            if prev is not None:
                deps.append((prev, True))
            sl[o.slot] = o
        final = []
        seen = self.seen[eng]
        sd = self.seen_dma[eng]
        for d, raw in deps:
            if d.dma:
                key = (d.pool, d.slot)
                if sd.get(key, 0) >= d.val:
                    continue
                sd[key] = d.val
                final.append(d)
            else:
                if d.eng == eng and not raw and not dma:
                    continue
                if seen[d.eng] >= d.idx:
                    continue
                seen[d.eng] = d.idx
                d.signal = True
                final.append(d)
        o.deps = final
        for t in reads:
            if dma:
                t.rd.append(o)
            else:
                t.r[eng] = o
        for t in writes:
            t.w = o
            t.r = {}
            t.rd = []
        self.ops[eng].append(o)
        if is_out:
            self.out_dmas.append(o)
        return o

    def emit(self):
        nc = self.nc
        if self.out_dmas:
            o = Op()
            o.eng = "sp"
            o.fn = None
            o.idx = len(self.ops["sp"])
            o.signal = False
            o.dma = False
            o.cnt = 0
            o.deps = list(self.out_dmas)
            self.ops["sp"].append(o)
        prog_sem = {e: nc.alloc_semaphore("ps_" + e) for e in self.ENGS}
        slot_sem = {p: [nc.alloc_semaphore("ds_%s_%d" % (p, i)) for i in range(NSLOT)]
                    for p in self.ndma}
        for e in self.ENGS:
            c = 0
            for o in self.ops[e]:
                if o.signal and not o.dma:
                    c += 1
                o.cnt = c

        def run(ename, eng):
            for o in self.ops[ename]:
                for d in o.deps:
                    if d.dma:
                        eng.wait_ge(slot_sem[d.pool][d.slot], d.val)
                    else:
                        eng.wait_ge(prog_sem[d.eng], d.cnt)
                if o.fn is None:
                    continue
                ins = o.fn(eng)
                if o.dma:
                    ins.then_inc(slot_sem[o.pool][o.slot], o.inc)
                elif o.signal:
                    ins.then_inc(prog_sem[ename], 1)

        with nc.Block() as block:
            @block.tensor
            def _(e):
                run("pe", e)

            @block.scalar
            def _(e):
                run("act", e)

            @block.vector
            def _(e):
                run("dve", e)

            @block.gpsimd
            def _(e):
                run("pool", e)

            @block.sync
            def _(e):
                run("sp", e)

    def dma(self, eng, out, in_, reads=(), writes=(), is_out=False):
        return self.op(eng, lambda e: e.dma_start(out=out, in_=in_), reads, writes,
                       dma=True, is_out=is_out)

    def cc(self, in_ap, out_ap, groups, reads=(), writes=()):
        return self.op("pool", lambda e: e.collective_compute(
            "AllGather", ALU.bypass, replica_groups=groups, ins=[in_ap], outs=[out_ap]),
            reads, writes, dma=True, pool="cc", inc=1)

    def mm(self, out, lhsT, rhs, start, stop, reads=(), writes=()):
        return self.op("pe", lambda e: e.matmul(out, lhsT, rhs, start=start, stop=stop),
                       reads, writes)

    def act(self, out, in_, func, reads=(), writes=(), **kw):
        return self.op("act", lambda e: e.activation(out=out, in_=in_, func=func, **kw), reads, writes)

    def tt(self, eng, out, in0, in1, op, reads=(), writes=()):
        return self.op(eng, lambda e: e.tensor_tensor(out=out, in0=in0, in1=in1, op=op), reads, writes)

    def ts(self, eng, out, in0, s1, s2, op0, op1=None, reads=(), writes=()):
        if op1 is None:
            return self.op(eng, lambda e: e.tensor_scalar(out=out, in0=in0, scalar1=s1, scalar2=None, op0=op0),
                           reads, writes)
        return self.op(eng, lambda e: e.tensor_scalar(out=out, in0=in0, scalar1=s1, scalar2=s2, op0=op0, op1=op1),
                       reads, writes)

    def stt(self, eng, out, in0, scalar, in1, op0, op1, reads=(), writes=()):
        return self.op(eng, lambda e: e.scalar_tensor_tensor(out=out, in0=in0, scalar=scalar, in1=in1,
                                                             op0=op0, op1=op1), reads, writes)

    def alloc_psum(self, n=8):
        for i in range(n):
            h = self.nc.alloc_psum_tensor("psb%d" % i, [128, 512], F32)
            self.psum.append((h, T("psum%d" % i)))

    def next_psum(self):
        r = self.psum[self.psum_i % len(self.psum)]
        self.psum_i += 1
        return r

    def sb(self, name, shape, dt):
        return self.nc.alloc_sbuf_tensor(name, shape, dt), T(name)


def new_nc():
    return bass.Bass("TRN2", target_bir_lowering=False)


def din(nc, name, shape, dt=F32):
    return nc.dram_tensor(name, list(shape), dt, kind="ExternalInput").ap()


def dout(nc, name, shape, dt=F32):
    return nc.dram_tensor(name, list(shape), dt, kind="ExternalOutput").ap()


def dint(nc, name, shape, dt=F32):
    return nc.dram_tensor(name, list(shape), dt, kind="Internal").ap()


def wchunks(N, rows_per_rank):
    cw_max = max(256, (4 * 1024 * 1024 // 4 // rows_per_rank) // 256 * 256)
    out = []
    c0 = 0
    while c0 < N:
        cw = min(cw_max, N - c0)
        out.append((c0, cw))
        c0 += cw
    return out


def host_wshards(W):
    K, N = W.shape
    rp = K // NC
    ch = wchunks(N, rp)
    res = []
    for r in range(NC):
        blk = W[r * rp:(r + 1) * rp]
        res.append(np.concatenate([np.ascontiguousarray(blk[:, c0:c0 + cw]).ravel() for c0, cw in ch]))
    return res


class GW:
    pass


def ag_weight(P, nc, name, K, N):
    rp = K // NC
    ch = wchunks(N, rp)
    ext = din(nc, name, [rp * N])
    g = GW()
    g.K = K
    g.N = N
    g.chunks = []
    off = 0
    for i, (c0, cw) in enumerate(ch):
        bn = dint(nc, "%s_b%d" % (name, i), [rp, cw])
        gt = dint(nc, "%s_g%d" % (name, i), [K, cw])
        tb = T()
        tg = T()
        src = ext[off:off + rp * cw].rearrange("(r c) -> r c", c=cw)
        P.dma("sp", bn, src, writes=[tb])
        P.cc(bn, gt, [list(range(NC))], reads=[tb], writes=[tg])
        g.chunks.append((c0, cw, gt, tg))
        off += rp * cw
    return g


CB = 256
KG = 32


class WRing:
    def __init__(self, P, nb=3, kg=KG):
        self.P = P
        self.kg = kg
        self.bufs = [P.sb("wring%d" % i, [128, kg, CB], BF16) for i in range(nb)]
        self.i = 0

    def load(self, src_ap, src_t, kcs, cbw):
        b, t = self.bufs[self.i % len(self.bufs)]
        self.i += 1
        self.P.dma("pool", b[:, 0:kcs, 0:cbw], src_ap, reads=[src_t], writes=[t])
        return b, t


def gemm(P, ring, gw, actT, act_t, tok_blocks, epi, col_lo=0, col_hi=None, act_kc0=0):
    K = gw.K
    KC = K // 128
    col_hi = gw.N if col_hi is None else col_hi
    kgs = [(k0, min(ring.kg, KC - k0)) for k0 in range(0, KC, ring.kg)]
    tiles = []
    for (c0, cw, ap, tg) in gw.chunks:
        lo = max(c0, col_lo)
        hi = min(c0 + cw, col_hi)
        c = lo
        while c < hi:
            w = min(CB, hi - c)
            for (k0, kn) in kgs:
                v = ap.rearrange("(kc p) n -> p kc n", p=128)[:, k0:k0 + kn, c - c0:c - c0 + w]
                tiles.append((c, w, k0, kn, v, tg))
            c += w
    NB = len(ring.bufs)
    loaded = []

    def issue(i):
        c, w, k0, kn, v, tg = tiles[i]
        loaded.append(ring.load(v, tg, kn, w))

    for i in range(min(NB - 1, len(tiles))):
        issue(i)
    ti = 0
    ncolblk = len(tiles) // len(kgs)
    for cbi in range(ncolblk):
        c, w = tiles[ti][0], tiles[ti][1]
        nmc = w // 128
        banks = {}
        for gi, (k0, kn) in enumerate(kgs):
            if ti + NB - 1 < len(tiles):
                issue(ti + NB - 1)
            b, t = loaded[ti]
            ti += 1
            for mc in range(nmc):
                for tbi, (t0, tw) in enumerate(tok_blocks):
                    if gi == 0:
                        banks[(mc, tbi)] = P.next_psum()
                    ps, pt = banks[(mc, tbi)]
                    for kc in range(kn):
                        P.mm(ps[:, 0:tw], b[:, kc, mc * 128:(mc + 1) * 128],
                             actT[:, act_kc0 + k0 + kc, t0:t0 + tw],
                             gi == 0 and kc == 0, gi == len(kgs) - 1 and kc == kn - 1,
                             reads=[t, act_t], writes=[pt])
                    if gi == len(kgs) - 1:
                        epi(c + mc * 128, tbi, (t0, tw), ps, pt)


D = 4096
KC = 32
TT = 1024
HALO = 32
TH = TT + HALO
EPS = 1e-6


def norm_mod(P, src, ntok, A, B, a_t, hT, h_t, hoff, ones, ones_t, xt, xt_t, scr):
    sq0, sq0_t, rstd, rstd_t, tmp0, tmp0_t = scr
    sqs = [(sq0, sq0_t), P.sb("nm_sq1", [128, 256], F32)]
    tmps = [(tmp0, tmp0_t), P.sb("nm_tmp1", [128, 256], F32)]
    sv = src.rearrange("(kc p) t -> p kc t", p=128)
    t0 = 0
    while t0 < ntok:
        tw = min(256, ntok - t0)
        P.dma("sp", xt[:, :, 0:tw], sv[:, :, t0:t0 + tw], writes=[xt_t])
        ps, pt = P.next_psum()
        for kc in range(KC):
            sq, sq_t = sqs[kc % 2]
            P.act(sq[:, 0:tw], xt[:, kc, 0:tw], AF.Square, reads=[xt_t], writes=[sq_t])
            P.mm(ps[:, 0:tw], ones[:, :], sq[:, 0:tw], kc == 0, kc == KC - 1, reads=[ones_t, sq_t], writes=[pt])
        P.ts("dve", rstd[:, 0:tw], ps[:, 0:tw], 1.0 / D, EPS, ALU.mult, ALU.add, reads=[pt], writes=[rstd_t])
        P.act(rstd[:, 0:tw], rstd[:, 0:tw], AF.Sqrt, reads=[rstd_t], writes=[rstd_t])
        P.op("dve", lambda e, tw=tw: e.reciprocal(out=rstd[:, 0:tw], in_=rstd[:, 0:tw]), reads=[rstd_t], writes=[rstd_t])
        for kc in range(KC):
            tmp, tmp_t = tmps[kc % 2]
            P.tt("dve", tmp[:, 0:tw], xt[:, kc, 0:tw], rstd[:, 0:tw], ALU.mult, reads=[xt_t, rstd_t], writes=[tmp_t])
            P.act(hT[:, kc, hoff + t0:hoff + t0 + tw], tmp[:, 0:tw], AF.Identity, reads=[tmp_t, a_t], writes=[h_t],
                  scale=A[:, kc:kc + 1], bias=B[:, kc:kc + 1])
        t0 += tw


def build_l0in():
    nc = new_nc()
    P = Prog(nc)
    P.alloc_psum(8)
    xT = din(nc, "xT", [D, TH])
    sc_in = din(nc, "sc", [128, KC])
    sh_in = din(nc, "sh", [128, KC])
    ng_in = din(nc, "ng", [128, KC])
    cw_in = din(nc, "cw", [128, 16 * 31])
    cp_in = din(nc, "cp", [128, 48])
    halo_in = din(nc, "halo", [128, 1])
    cos_in = din(nc, "cos", [128, TT])
    sin_in = din(nc, "sin", [128, TT])
    rot_in = din(nc, "rotm", [128, 128])
    gw = rep_weight(din(nc, "w_in", [D, 10240]), D, 10240)
    yaT = dout(nc, "yaT", [2048, TT], BF16)
    qT = dout(nc, "qT", [2048, TT], BF16)
    kT = dout(nc, "kT", [2048, TT], BF16)
    vT = dout(nc, "vT", [2048, TT], BF16)
    co_d = dint(nc, "co_d", [2048, TT])

    def ld(name, ap, shape):
        b, t = P.sb(name, shape, F32)
        P.dma("sp", b[:, :], ap, writes=[t])
        return b, t

    sc, sc_t = ld("sc_s", sc_in, [128, KC])
    sh, sh_t = ld("sh_s", sh_in, [128, KC])
    ng, ng_t = ld("ng_s", ng_in, [128, KC])
    cw, cw_t = ld("cw_s", cw_in, [128, 16 * 31])
    cp, cp_t = ld("cp_s", cp_in, [128, 48])
    halo, halo_t = ld("halo_s", halo_in, [128, 1])
    cos, cos_t = ld("cos_s", cos_in, [128, TT])
    sin, sin_t = ld("sin_s", sin_in, [128, TT])
    rotm, rot_t = ld("rot_s", rot_in, [128, 128])
    ones, ones_t = P.sb("ones", [128, 128], F32)
    P.op("dve", lambda e: e.memset(ones[:, :], 1.0), writes=[ones_t])
    A, a_t = P.sb("A", [128, KC], F32)
    P.ts("dve", A[:, :], sc[:, :], 1.0, None, ALU.add, reads=[sc_t], writes=[a_t])
    P.tt("dve", A[:, :], A[:, :], ng[:, :], ALU.mult, reads=[a_t, ng_t], writes=[a_t])
    hT, h_t = P.sb("hT", [128, KC, TH], BF16)
    xt, xt_t = P.sb("xt", [128, KC, 256], F32)
    scr = P.sb("sq", [128, 256], F32) + P.sb("rstd", [128, 256], F32) + P.sb("tmpn", [128, 256], F32)
    P.op("act", lambda e: e.activation(out=sh[:, :], in_=sh[:, :], func=AF.Identity), reads=[sh_t, a_t], writes=[a_t, sh_t])
    norm_mod(P, xT, TH, A, sh, a_t, hT, h_t, 0, ones, ones_t, xt, xt_t, scr)

    ring = WRing(P, 3)
    own = [(HALO, 512), (HALO + 512, 512)]
    allb = [(0, HALO), (HALO, 512), (HALO + 512, 512)]
    ob = [P.sb("ob%d" % i, [128, 512], BF16) for i in range(2)]
    obi = [0]
    qf = [P.sb("qf%d" % i, [128, 512], F32) for i in range(2)]
    t1 = [P.sb("t1_%d" % i, [128, 512], F32) for i in range(2)]
    t2 = [P.sb("t2_%d" % i, [128, 512], F32) for i in range(2)]
    qi = [0]

    def epi_qk(col0, tbi, tb, ps, pt):
        t0, tw = tb
        tl = t0 - HALO
        i = qi[0] % 2
        qi[0] += 1
        q, q_t = qf[i]
        a1, a1_t = t1[i]
        a2, a2_t = t2[i]
        P.act(q[:, :], ps[:, :], AF.Copy, reads=[pt], writes=[q_t])
        ps2, pt2 = P.next_psum()
        P.mm(ps2[:, :], rotm[:, :], q[:, :], True, True, reads=[rot_t, q_t], writes=[pt2])
        P.tt("dve", a1[:, :], q[:, :], cos[:, tl:tl + tw], ALU.mult, reads=[q_t, cos_t], writes=[a1_t])
        P.tt("dve", a2[:, :], ps2[:, :], sin[:, tl:tl + tw], ALU.mult, reads=[pt2, sin_t], writes=[a2_t])
        o, o_t = ob[obi[0] % 2]
        obi[0] += 1
        P.tt("pool", o[:, :], a1[:, :], a2[:, :], ALU.add, reads=[a1_t, a2_t], writes=[o_t])
        dst = qT if col0 < 6144 else kT
        r0 = (col0 - 4096) % 2048
        P.dma("sp", dst[r0:r0 + 128, tl:tl + tw], o[:, :], reads=[o_t], is_out=True)

    gemm(P, ring, gw, hT, h_t, own, epi_qk, 4096, 8192)

    def epi_v(col0, tbi, tb, ps, pt):
        t0, tw = tb
        tl = t0 - HALO
        o, o_t = ob[obi[0] % 2]
        obi[0] += 1
        P.act(o[:, :], ps[:, :], AF.Copy, reads=[pt], writes=[o_t])
        r0 = col0 - 8192
        P.dma("sp", vT[r0:r0 + 128, tl:tl + tw], o[:, :], reads=[o_t], is_out=True)

    gemm(P, ring, gw, hT, h_t, own, epi_v, 8192, 10240)

    sg, sg_t = P.sb("sg", [128, TH], F32)
    U = [P.sb("U%d" % i, [128, TH], F32) for i in range(2)]
    CO = [P.sb("CO%d" % i, [128, TT], F32) for i in range(2)]
    sq2, sq2_t = P.sb("sq2", [128, 512], F32)
    S1 = [P.next_psum() for _ in range(2)]
    S2 = [P.next_psum() for _ in range(2)]
    P.psum = P.psum[4:] if False else P.psum
    gem_banks = [b for b in P.psum if b not in S1 and b not in S2]
    save = P.psum
    P.psum = gem_banks
    P.psum_i = 0
    cod_t = [T() for _ in range(16)]
    for cc in range(16):
        u, u_t = U[cc % 2]
        co, co_t = CO[cc % 2]

        def epi_g(col0, tbi, tb, ps, pt):
            t0, tw = tb
            P.act(sg[:, t0:t0 + tw], ps[:, 0:tw], AF.Sigmoid, reads=[pt], writes=[sg_t])

        def epi_a(col0, tbi, tb, ps, pt, u=u, u_t=u_t):
            t0, tw = tb
            P.tt("dve", u[:, t0:t0 + tw], ps[:, 0:tw], sg[:, t0:t0 + tw], ALU.mult, reads=[pt, sg_t], writes=[u_t])

        gemm(P, ring, gw, hT, h_t, allb, epi_g, 2048 + cc * 128, 2048 + (cc + 1) * 128)
        gemm(P, ring, gw, hT, h_t, allb, epi_a, cc * 128, (cc + 1) * 128)
        P.ts("dve", u[:, 0:HALO], u[:, 0:HALO], halo[:, 0:1], None, ALU.mult, reads=[u_t, halo_t], writes=[u_t])
        for hf, eng in ((0, "dve"), (1, "dve")):
            o0 = hf * 512
            P.ts(eng, co[:, o0:o0 + 512], u[:, o0 + 2:o0 + 2 + 512], cw[:, cc * 31:cc * 31 + 1], cp[:, cc:cc + 1],
                 ALU.mult, ALU.add, reads=[u_t, cw_t, cp_t], writes=[co_t])
            for k in range(1, 31):
                P.stt(eng, co[:, o0:o0 + 512], u[:, o0 + k + 2:o0 + k + 2 + 512], cw[:, cc * 31 + k:cc * 31 + k + 1],
                      co[:, o0:o0 + 512], ALU.mult, ALU.add, reads=[u_t, co_t], writes=[co_t])
        for hf in range(2):
            o0 = hf * 512
            P.mm(S1[hf][0][:, :], ones[:, :], co[:, o0:o0 + 512], cc == 0, cc == 15, reads=[ones_t, co_t], writes=[S1[hf][1]])
            P.act(sq2[:, :], co[:, o0:o0 + 512], AF.Square, reads=[co_t], writes=[sq2_t])
            P.mm(S2[hf][0][:, :], ones[:, :], sq2[:, :], cc == 0, cc == 15, reads=[ones_t, sq2_t], writes=[S2[hf][1]])
        P.dma("sp", co_d[cc * 128:(cc + 1) * 128, :], co[:, :], reads=[co_t], writes=[cod_t[cc]])
    P.psum = save
    mu, mu_t = P.sb("mu", [128, TT], F32)
    rs, rs_t = P.sb("rs", [128, TT], F32)
    for hf in range(2):
        o0 = hf * 512
        P.ts("dve", mu[:, o0:o0 + 512], S1[hf][0][:, :], 1.0 / 2048, None, ALU.mult, reads=[S1[hf][1]], writes=[mu_t])
        P.ts("dve", rs[:, o0:o0 + 512], S2[hf][0][:, :], 1.0 / 2048, None, ALU.mult, reads=[S2[hf][1]], writes=[rs_t])
    m2, m2_t = sg, sg_t
    P.tt("dve", m2[:, 0:TT], mu[:, :], mu[:, :], ALU.mult, reads=[mu_t], writes=[m2_t])
    P.tt("dve", rs[:, :], rs[:, :], m2[:, 0:TT], ALU.subtract, reads=[rs_t, m2_t], writes=[rs_t])
    P.ts("dve", rs[:, :], rs[:, :], EPS, None, ALU.add, reads=[rs_t], writes=[rs_t])
    P.act(rs[:, :], rs[:, :], AF.Sqrt, reads=[rs_t], writes=[rs_t])
    P.op("dve", lambda e: e.reciprocal(out=rs[:, :], in_=rs[:, :]), reads=[rs_t], writes=[rs_t])
    co_all_t = T()
    for cc in range(16):
        co, co_t = CO[cc % 2]
        P.dma("sp", co[:, :], co_d[cc * 128:(cc + 1) * 128, :], reads=[cod_t[cc]], writes=[co_t])
        P.tt("dve", co[:, :], co[:, :], mu[:, :], ALU.subtract, reads=[co_t, mu_t], writes=[co_t])
        P.tt("dve", co[:, :], co[:, :], rs[:, :], ALU.mult, reads=[co_t, rs_t], writes=[co_t])
        for hf in range(2):
            o, o_t = ob[obi[0] % 2]
            obi[0] += 1
            P.act(o[:, :], co[:, hf * 512:(hf + 1) * 512], AF.Silu, reads=[co_t, cp_t], writes=[o_t],
                  scale=cp[:, 16 + cc:17 + cc], bias=cp[:, 32 + cc:33 + cc])
            P.dma("sp", yaT[cc * 128:(cc + 1) * 128, hf * 512:(hf + 1) * 512], o[:, :], reads=[o_t], is_out=True)
    P.emit()
    return nc


def build_ada():
    nc = new_nc()
    P = Prog(nc)
    P.alloc_psum(8)
    cT_in = din(nc, "cT", [128, KC * 2])
    ada = din(nc, "ada_s", [12, D, 512])
    adab_in = din(nc, "adab", [128, 48])
    modp = dout(nc, "modp", [128, 96])
    cT, c_t = P.sb("cT_s", [128, KC * 2], F32)
    P.dma("sp", cT[:, :], cT_in, writes=[c_t])
    ab, ab_t = P.sb("ab", [128, 48], F32)
    P.dma("sp", ab[:, :], adab_in, writes=[ab_t])
    P.act(cT[:, :], cT[:, :], AF.Silu, reads=[c_t], writes=[c_t])
    wt = [P.sb("wt%d" % i, [128, KC, 256], F32) for i in range(2)]
    res, res_t = P.sb("res", [128, 96], F32)
    i = 0
    for g in range(12):
        for half in range(2):
            w, w_t = wt[i % 2]
            i += 1
            P.dma("sp", w[:, :, :], ada[g].rearrange("(kc p) n -> p kc n", p=128)[:, :, half * 256:(half + 1) * 256],
                  writes=[w_t])
            for c2 in range(2):
                ci = half * 2 + c2
                ps, pt = P.next_psum()
                for kc in range(KC):
                    P.mm(ps[:, 0:2], w[:, kc, c2 * 128:(c2 + 1) * 128], cT[:, kc * 2:kc * 2 + 2], kc == 0, kc == KC - 1,
                         reads=[w_t, c_t], writes=[pt])
                P.ts("dve", res[:, (g * 4 + ci) * 2:(g * 4 + ci) * 2 + 2], ps[:, 0:2], ab[:, g * 4 + ci:g * 4 + ci + 1], None,
                     ALU.add, reads=[pt, ab_t], writes=[res_t])
    P.dma("sp", modp, res[:, :], reads=[res_t], is_out=True)
    P.emit()
    return nc


def build_attn():
    S = 4096
    nc = new_nc()
    P = Prog(nc)
    P.alloc_psum(8)
    qT_in = din(nc, "qT", [4 * 128, S], BF16)
    kT_in = din(nc, "kT", [4 * 128, S], BF16)
    v_in = din(nc, "v", [S, 512], BF16)
    lam_in = din(nc, "lam", [1, 512])
    sub_in = din(nc, "subln", [128, 256])
    msk_in = din(nc, "mask", [4, 128, 512])
    yb = dout(nc, "yb", [S, 512])
    scale = 128.0 ** -0.5
    lam_init = 0.8 - 0.6 * 1.0
    qT, q_t = P.sb("qT_s", [128, 4, S], BF16)
    kT, k_t = P.sb("kT_s", [128, 4, S], BF16)
    for h in range(4):
        P.dma("sp", qT[:, h, :], qT_in[h * 128:(h + 1) * 128, :], writes=[q_t])
        P.dma("sp", kT[:, h, :], kT_in[h * 128:(h + 1) * 128, :], writes=[k_t])
    V, v_t = P.sb("V_s", [128, 32, 2, 260], BF16)
    P.op("dve", lambda e: e.memset(V[:, :, :, 256:257], 1.0), writes=[v_t])
    for hv in range(2):
        P.dma("sp", V[:, :, hv, 0:256], v_in.rearrange("(jb p) c -> p jb c", p=128)[:, :, hv * 256:(hv + 1) * 256],
              reads=[v_t], writes=[v_t])
    msk, m_t = P.sb("msk", [128, 4, 512], BF16)
    mskf, mf_t = P.sb("mskf", [128, 4, 512], F32)
    for o in range(4):
        P.dma("sp", mskf[:, o, :], msk_in[o], writes=[mf_t])
    P.op("dve", lambda e: e.tensor_copy(out=msk[:, :, :], in_=mskf[:, :, :]), reads=[mf_t], writes=[m_t])
    sub, sub_t = P.sb("sub", [128, 256], F32)
    P.dma("sp", sub[:, :], sub_in, writes=[sub_t])
    ones, ones_t = P.sb("ones", [128, 128], F32)
    P.op("dve", lambda e: e.memset(ones[:, :], 1.0), writes=[ones_t])
    onesb, onesb_t = P.sb("onesb", [128, 128], BF16)
    P.op("dve", lambda e: e.memset(onesb[:, :], 1.0), writes=[onesb_t])
    lm, lm_t = P.sb("lm", [1, 512], F32)
    P.dma("sp", lm[:, :], lam_in, writes=[lm_t])
    l2, l2_t = P.sb("l2", [1, 8], F32)
    pr, pr_t = P.sb("pr", [1, 256], F32)
    P.tt("dve", pr[:, 0:128], lm[:, 0:128], lm[:, 128:256], ALU.mult, reads=[lm_t], writes=[pr_t])
    P.tt("dve", pr[:, 128:256], lm[:, 256:384], lm[:, 384:512], ALU.mult, reads=[lm_t], writes=[pr_t])
    P.op("dve", lambda e: e.reduce_sum(out=l2[:, 0:1], in_=pr[:, 0:128], axis=AX.X), reads=[pr_t], writes=[l2_t])
    P.op("dve", lambda e: e.reduce_sum(out=l2[:, 1:2], in_=pr[:, 128:256], axis=AX.X), reads=[pr_t], writes=[l2_t])
    P.act(l2[:, 2:4], l2[:, 0:2], AF.Exp, reads=[l2_t], writes=[l2_t])
    P.tt("dve", l2[:, 4:5], l2[:, 2:3], l2[:, 3:4], ALU.subtract, reads=[l2_t], writes=[l2_t])
    P.ts("dve", l2[:, 5:6], l2[:, 4:5], lam_init, -1.0, ALU.add, ALU.mult, reads=[l2_t], writes=[l2_t])
    psl, psl_t = P.next_psum()
    P.mm(psl[:, 0:1], ones[0:1, :], l2[0:1, 5:6], True, True, reads=[ones_t, l2_t], writes=[psl_t])
    nlam, nlam_t = P.sb("nlam", [128, 1], F32)
    P.op("dve", lambda e: e.tensor_copy(out=nlam[:, :], in_=psl[:, 0:1]), reads=[psl_t], writes=[nlam_t])
    negm, negm_t = P.sb("negm", [128, 4], F32)
    mx, mx_t = P.sb("mx", [128, 16], F32)
    sqbs = [P.sb("sqb%d" % i, [128, 512], BF16) for i in range(2)]
    red, red_t = P.sb("red", [128, 8], F32)
    for h in range(4):
        for which, (src, s_t) in enumerate(((qT, q_t), (kT, k_t))):
            for blk in range(8):
                sqb, sqb_t = sqbs[blk % 2]
                P.tt("pool", sqb[:, :], src[:, h, blk * 512:(blk + 1) * 512], src[:, h, blk * 512:(blk + 1) * 512], ALU.mult,
                     reads=[s_t], writes=[sqb_t])
                ps, pt = P.next_psum()
                P.mm(ps[:, :], onesb[:, :], sqb[:, :], True, True, reads=[onesb_t, sqb_t], writes=[pt])
                P.op("dve", lambda e, ps=ps, blk=blk: e.reduce_max(out=red[:, blk:blk + 1], in_=ps[:, :], axis=AX.X),
                     reads=[pt], writes=[red_t])
            P.op("dve", lambda e, h=h, which=which: e.reduce_max(out=mx[:, h * 2 + which:h * 2 + which + 1], in_=red[:, 0:8], axis=AX.X),
                 reads=[red_t], writes=[mx_t])
        P.tt("dve", mx[:, 8 + h:9 + h], mx[:, h * 2:h * 2 + 1], mx[:, h * 2 + 1:h * 2 + 2], ALU.mult, reads=[mx_t], writes=[mx_t])
    P.ts("dve", mx[:, 8:12], mx[:, 8:12], 1.05, None, ALU.mult, reads=[mx_t], writes=[mx_t])
    P.act(mx[:, 12:16], mx[:, 8:12], AF.Sqrt, reads=[mx_t], writes=[mx_t])
    P.ts("dve", negm[:, :], mx[:, 12:16], -scale, None, ALU.mult, reads=[mx_t], writes=[negm_t])

    PT = [P.sb("PT%d" % i, [128, 512], BF16) for i in range(3)]
    pti = 0
    oacc = [P.sb("oacc%d" % i, [128, 4, 260], F32) for i in range(2)]
    on, on_t = P.sb("on", [128, 4, 256], F32)
    tmpo, tmpo_t = P.sb("tmpo", [128, 4, 256], F32)
    rl, rl_t = P.sb("rl", [128, 8], F32)
    ssq, ssq_t = P.sb("ssq", [128, 4], F32)
    junk, junk_t = P.sb("junk", [128, 256], F32)
    acc_banks = [P.next_psum() for _ in range(4)]
    rest = [b for b in P.psum if b not in acc_banks]
    for dh in range(2):
        for ib in range(8):
            for hh in range(2):
                h = dh * 2 + hh
                P.psum = rest
                njb = 4 * (ib + 1)
                for jb in range(njb):
                    ps, pt = P.next_psum()
                    P.mm(ps[:, :], kT[:, h, jb * 128:(jb + 1) * 128], qT[:, h, ib * 512:(ib + 1) * 512], True, True,
                         reads=[k_t, q_t], writes=[pt])
                    p, p_t = PT[pti % 3]
                    pti += 1
                    P.act(p[:, :], ps[:, :], AF.Exp, reads=[pt, negm_t], writes=[p_t], scale=scale, bias=negm[:, h:h + 1])
                    off = jb * 128 - ib * 512
                    if off >= 0:
                        P.tt("dve", p[:, :], p[:, :], msk[:, off // 128, :], ALU.mult, reads=[p_t, m_t], writes=[p_t])
                    for ii in range(4):
                        if off >= 0 and ii * 128 + 127 < off:
                            continue
                        first = jb == 0
                        last = (jb == ib * 4 + ii) if True else False
                        if jb > ib * 4 + ii:
                            continue
                        ab, ab_t = acc_banks[ii]
                        P.mm(ab[:, 0:257], p[:, ii * 128:(ii + 1) * 128], V[:, jb, dh, 0:257], first, last,
                             reads=[p_t, v_t], writes=[ab_t])
                oa, oa_t = oacc[hh]
                for ii in range(4):
                    ab, ab_t = acc_banks[ii]
                    P.act(oa[:, ii, 0:257], ab[:, 0:257], AF.Copy, reads=[ab_t], writes=[oa_t])
            o0, o0_t = oacc[0]
            o1, o1_t = oacc[1]
            for ii in range(4):
                P.op("dve", lambda e, ii=ii: e.reciprocal(out=rl[:, ii:ii + 1], in_=o0[:, ii, 256:257]), reads=[o0_t], writes=[rl_t])
                P.op("dve", lambda e, ii=ii: e.reciprocal(out=rl[:, 4 + ii:5 + ii], in_=o1[:, ii, 256:257]), reads=[o1_t], writes=[rl_t])
            P.ts("dve", rl[:, 4:8], rl[:, 4:8], nlam[:, 0:1], None, ALU.mult, reads=[rl_t, nlam_t], writes=[rl_t])
            for ii in range(4):
                P.ts("dve", on[:, ii, :], o0[:, ii, 0:256], rl[:, ii:ii + 1], None, ALU.mult, reads=[o0_t, rl_t], writes=[on_t])
                P.stt("dve", on[:, ii, :], o1[:, ii, 0:256], rl[:, 4 + ii:5 + ii], on[:, ii, :], ALU.mult, ALU.add,
                      reads=[o1_t, rl_t, on_t], writes=[on_t])
                P.act(junk[:, :], on[:, ii, :], AF.Square, reads=[on_t], writes=[junk_t])
                P.op("dve", lambda e, ii=ii: e.reduce_sum(out=ssq[:, ii:ii + 1], in_=junk[:, :], axis=AX.X), reads=[junk_t], writes=[ssq_t])
            P.ts("dve", ssq[:, :], ssq[:, :], 1.0 / 256, 1e-5, ALU.mult, ALU.add, reads=[ssq_t], writes=[ssq_t])
            P.act(ssq[:, :], ssq[:, :], AF.Sqrt, reads=[ssq_t], writes=[ssq_t])
            P.op("dve", lambda e: e.reciprocal(out=ssq[:, :], in_=ssq[:, :]), reads=[ssq_t], writes=[ssq_t])
            P.ts("dve", ssq[:, :], ssq[:, :], 1.0 - lam_init, None, ALU.mult, reads=[ssq_t], writes=[ssq_t])
            for ii in range(4):
                P.stt("dve", tmpo[:, ii, :], on[:, ii, :], ssq[:, ii:ii + 1], sub[:, :], ALU.mult, ALU.mult,
                      reads=[on_t, ssq_t, sub_t, tmpo_t], writes=[tmpo_t])
                r0 = ib * 512 + ii * 128
                P.dma("sp", yb[r0:r0 + 128, dh * 256:(dh + 1) * 256], tmpo[:, ii, :], reads=[tmpo_t], is_out=True)
    P.emit()
    return nc


def rep_weight(ap, K, N):
    g = GW()
    g.K = K
    g.N = N
    g.chunks = [(0, N, ap, T())]
    return g


TS = 512
NE = 8


def build_outffn(moe, FF):
    nc = new_nc()
    P = Prog(nc)
    P.alloc_psum(8)
    TL = 512
    xT = din(nc, "xT", [D, TT])
    yT = din(nc, "yT", [D, TT], BF16)
    md_in = din(nc, "md", [128, 6 * KC])
    w_out = din(nc, "w_out", [D, D])
    if moe:
        wr_in = din(nc, "wr", [D, NE])
        id_in = din(nc, "ident", [128, 128])
        wg = din(nc, "w_gate", [NE, D, FF])
        wu = din(nc, "w_up", [NE, D, FF])
        wd = din(nc, "w_down", [NE, FF, D])
    else:
        wg = din(nc, "w_gate", [D, FF])
        wu = din(nc, "w_up", [D, FF])
        wd = din(nc, "w_down", [FF, D])
    x2T = dout(nc, "x2T", [D, TT])
    xres = dint(nc, "xres", [D, TT])
    gwo = rep_weight(w_out, D, D)
    md, md_t = P.sb("md_s", [128, 6 * KC], F32)
    P.dma("sp", md[:, :], md_in, writes=[md_t])
    ones, ones_t = P.sb("ones", [128, 128], F32)
    P.op("dve", lambda e: e.memset(ones[:, :], 1.0), writes=[ones_t])
    A, a_t = P.sb("A", [128, KC], F32)
    P.ts("dve", A[:, :], md[:, 2 * KC:3 * KC], 1.0, None, ALU.add, reads=[md_t], writes=[a_t])
    P.tt("dve", A[:, :], A[:, :], md[:, 4 * KC:5 * KC], ALU.mult, reads=[a_t, md_t], writes=[a_t])
    actT, act_t = P.sb("actT", [128, KC, TS], BF16)
    ring = WRing(P, 4, 16)
    xb = [P.sb("xb%d" % i, [128, 512], F32) for i in range(3)]
    xo = [P.sb("xo%d" % i, [128, 512], F32) for i in range(3)]
    cnt = [0]
    sqs = [P.sb("sq%d" % i, [128, TL], F32) for i in range(2)]
    tmps = [P.sb("tmpn%d" % i, [128, 128], F32) for i in range(2)]
    rstd, rstd_t = P.sb("rstd", [128, TL], F32)
    aT, aT_t = P.sb("aT", [128, FF // 128, TL], BF16)
    if moe:
        wr, wr_t = P.sb("wr_s", [128, KC, NE], F32)
        P.dma("sp", wr[:, :, :], wr_in.rearrange("(kc p) e -> p kc e", p=128), writes=[wr_t])
        ident, id_t = P.sb("ident_s", [128, 128], F32)
        P.dma("sp", ident[:, :], id_in, writes=[id_t])
        h32 = [P.sb("h32_%d" % i, [128, 128], F32) for i in range(2)]
        gts, g_t = P.sb("gts", [128, TS // 128, NE], F32)
        lg, lg2, eq1, eq2 = [P.sb(n, [128, NE], F32)[0] for n in ("lg", "lg2", "eq1", "eq2")]
        m1 = P.sb("m1", [128, 8], F32)[0]
        sc_t = T("gscratch")
        bcs = [P.sb("bc%d" % i, [128, 128], F32) for i in range(2)]
        Gs = [P.sb("G%d" % i, [128, TL], F32) for i in range(2)]
        tmpu = [P.sb("tmpu%d" % i, [128, TL], F32) for i in range(2)]
        acc, acc_t = P.sb("acc", [128, KC, TL], F32)
        xt, xt_t = acc, acc_t
    else:
        xt, xt_t = P.sb("xt", [128, KC, 128], F32)
    yv = yT.rearrange("(kc p) t -> p kc t", p=128)
    sv = xres.rearrange("(kc p) t -> p kc t", p=128)
    k2 = [0]
    for s in range(TT // TS):
        s0 = s * TS
        xres_t = [T() for _ in range(KC)]
        for q4 in range(4):
            P.dma("sp", actT[:, q4 * 8:(q4 + 1) * 8, :], yv[:, q4 * 8:(q4 + 1) * 8, s0:s0 + TS], reads=[act_t], writes=[act_t])

        def epi_o(col0, tbi, tb, ps, pt, s0=s0, xres_t=xres_t):
            t0, tw = tb
            c = col0 // 128
            i = cnt[0] % 3
            cnt[0] += 1
            x, x_t = xb[i]
            o, o_t = xo[i]
            P.dma("sp", x[:, 0:tw], xT[col0:col0 + 128, s0 + t0:s0 + t0 + tw], writes=[x_t])
            P.stt("dve", o[:, 0:tw], ps[:, 0:tw], md[:, c:c + 1], x[:, 0:tw], ALU.mult, ALU.add, reads=[pt, md_t, x_t], writes=[o_t])
            P.dma("sp", xres[col0:col0 + 128, s0 + t0:s0 + t0 + tw], o[:, 0:tw], reads=[o_t], writes=[xres_t[c]])

        gemm(P, ring, gwo, actT, act_t, [(0, TS)], epi_o)
        for t0 in range(0, TS, 128):
            tw = 128
            P.dma("sp", xt[:, :, 0:tw], sv[:, :, s0 + t0:s0 + t0 + tw], reads=xres_t, writes=[xt_t])
            ps, pt = P.next_psum()
            for kc in range(KC):
                sq, sq_t = sqs[kc % 2]
                P.act(sq[:, 0:tw], xt[:, kc, 0:tw], AF.Square, reads=[xt_t], writes=[sq_t])
                P.mm(ps[:, 0:tw], ones[:, :], sq[:, 0:tw], kc == 0, kc == KC - 1, reads=[ones_t, sq_t], writes=[pt])
            P.ts("dve", rstd[:, 0:tw], ps[:, 0:tw], 1.0 / D, EPS, ALU.mult, ALU.add, reads=[pt], writes=[rstd_t])
            P.act(rstd[:, 0:tw], rstd[:, 0:tw], AF.Sqrt, reads=[rstd_t], writes=[rstd_t])
            P.op("dve", lambda e, tw=tw: e.reciprocal(out=rstd[:, 0:tw], in_=rstd[:, 0:tw]), reads=[rstd_t], writes=[rstd_t])
            if moe:
                lps, lpt = P.next_psum()
            for kc in range(KC):
                tmp, tmp_t = tmps[kc % 2]
                P.tt("dve", tmp[:, 0:tw], xt[:, kc, 0:tw], rstd[:, 0:tw], ALU.mult, reads=[xt_t, rstd_t], writes=[tmp_t])
                if moe:
                    h, h_t = h32[kc % 2]
                    P.act(h[:, :], tmp[:, 0:tw], AF.Identity, reads=[tmp_t, a_t, md_t], writes=[h_t],
                          scale=A[:, kc:kc + 1], bias=md[:, KC + kc:KC + kc + 1])
                    P.op("pool", lambda e, h=h, kc=kc, t0=t0, tw=tw: e.tensor_copy(out=actT[:, kc, t0:t0 + tw], in_=h[:, :]),
                         reads=[h_t], writes=[act_t])
                    P.mm(lps[:, 0:NE], h[:, :], wr[:, kc, :], kc == 0, kc == KC - 1, reads=[h_t, wr_t], writes=[lpt])
                else:
                    P.act(actT[:, kc, t0:t0 + tw], tmp[:, 0:tw], AF.Identity, reads=[tmp_t, a_t, md_t], writes=[act_t],
                          scale=A[:, kc:kc + 1], bias=md[:, KC + kc:KC + kc + 1])
            if moe:
                tb = t0 // 128
                rw = dict(reads=[sc_t], writes=[sc_t])
                P.op("dve", lambda e, lps=lps: e.tensor_copy(out=lg[:, :], in_=lps[:, 0:NE]), reads=[lpt, sc_t], writes=[sc_t])
                P.op("dve", lambda e: e.reduce_max(out=m1[:, 0:1], in_=lg[:, :], axis=AX.X), **rw)
                P.ts("dve", eq1[:, :], lg[:, :], m1[:, 0:1], None, ALU.is_equal, **rw)
                P.ts("dve", lg2[:, :], eq1[:, :], -1e30, None, ALU.mult, **rw)
                P.tt("dve", lg2[:, :], lg2[:, :], lg[:, :], ALU.add, **rw)
                P.op("dve", lambda e: e.reduce_max(out=m1[:, 1:2], in_=lg2[:, :], axis=AX.X), **rw)
                P.ts("dve", eq2[:, :], lg2[:, :], m1[:, 1:2], None, ALU.is_equal, **rw)
                P.tt("dve", m1[:, 2:3], m1[:, 1:2], m1[:, 0:1], ALU.subtract, **rw)
                P.act(m1[:, 3:4], m1[:, 2:3], AF.Exp, **rw)
                P.ts("dve", m1[:, 4:5], m1[:, 3:4], 1.0, None, ALU.add, **rw)
                P.op("dve", lambda e: e.reciprocal(out=m1[:, 5:6], in_=m1[:, 4:5]), **rw)
                P.tt("dve", m1[:, 6:7], m1[:, 3:4], m1[:, 5:6], ALU.mult, **rw)
                P.ts("dve", gts[:, tb, :], eq1[:, :], m1[:, 5:6], None, ALU.mult, reads=[sc_t, g_t], writes=[g_t])
                P.stt("dve", gts[:, tb, :], eq2[:, :], m1[:, 6:7], gts[:, tb, :], ALU.mult, ALU.add, reads=[sc_t, g_t], writes=[g_t])
        for tl in range(TS // TL):
            tt0 = tl * TL
            g0 = s0 + tt0
            if not moe:
                def epi_g(col0, tbi, tb, ps, pt):
                    P.act(aT[:, col0 // 128, :], ps[:, 0:TL], AF.Silu, reads=[pt], writes=[aT_t])

                def epi_u(col0, tbi, tb, ps, pt):
                    P.tt("dve", aT[:, col0 // 128, :], ps[:, 0:TL], aT[:, col0 // 128, :], ALU.mult, reads=[pt, aT_t], writes=[aT_t])

                def epi_d(col0, tbi, tb, ps, pt, g0=g0, xres_t=xres_t):
                    c = col0 // 128
                    i = cnt[0] % 3
                    cnt[0] += 1
                    x, x_t = xb[i]
                    o, o_t = xo[i]
                    P.dma("sp", x[:, 0:TL], xres[col0:col0 + 128, g0:g0 + TL], reads=[xres_t[c]], writes=[x_t])
                    P.stt("dve", o[:, 0:TL], ps[:, 0:TL], md[:, 3 * KC + c:3 * KC + c + 1], x[:, 0:TL], ALU.mult, ALU.add,
                          reads=[pt, md_t, x_t], writes=[o_t])
                    P.dma("sp", x2T[col0:col0 + 128, g0:g0 + TL], o[:, 0:TL], reads=[o_t], is_out=True)

                gemm(P, ring, rep_weight(wg, D, FF), actT, act_t, [(tt0, TL)], epi_g)
                gemm(P, ring, rep_weight(wu, D, FF), actT, act_t, [(tt0, TL)], epi_u)
                gemm(P, ring, rep_weight(wd, FF, D), aT, aT_t, [(0, TL)], epi_d)
                continue
            for ex in range(NE):
                G, G_t = Gs[ex % 2]
                for bi in range(TL // 128):
                    tb = tt0 // 128 + bi
                    bc, bc_t = bcs[k2[0] % 2]
                    k2[0] += 1
                    P.ts("dve", bc[:, :], ones[:, :], gts[:, tb, ex:ex + 1], None, ALU.mult, reads=[ones_t, g_t], writes=[bc_t])
                    ps, pt = P.next_psum()
                    P.mm(ps[:, 0:128], bc[:, :], ident[:, :], True, True, reads=[bc_t, id_t], writes=[pt])
                    P.act(G[:, bi * 128:(bi + 1) * 128], ps[:, 0:128], AF.Copy, reads=[pt], writes=[G_t])

                def epi_g(col0, tbi, tb, ps, pt):
                    P.act(aT[:, col0 // 128, :], ps[:, 0:TL], AF.Silu, reads=[pt], writes=[aT_t])

                def epi_u(col0, tbi, tb, ps, pt, ex=ex, G=G, G_t=G_t):
                    c = col0 // 128
                    u, u_t = tmpu[k2[0] % 2]
                    k2[0] += 1
                    P.tt("dve", u[:, :], ps[:, 0:TL], G[:, :], ALU.mult, reads=[pt, G_t], writes=[u_t])
                    P.tt("pool", aT[:, c, :], u[:, :], aT[:, c, :], ALU.mult, reads=[u_t, aT_t], writes=[aT_t])

                def epi_d(col0, tbi, tb, ps, pt, ex=ex):
                    c = col0 // 128
                    if ex == 0:
                        P.act(acc[:, c, :], ps[:, 0:TL], AF.Copy, reads=[pt], writes=[acc_t])
                    else:
                        P.tt("dve", acc[:, c, :], acc[:, c, :], ps[:, 0:TL], ALU.add, reads=[pt, acc_t], writes=[acc_t])

                gemm(P, ring, rep_weight(wg[ex], D, FF), actT, act_t, [(tt0, TL)], epi_g)
                gemm(P, ring, rep_weight(wu[ex], D, FF), actT, act_t, [(tt0, TL)], epi_u)
                gemm(P, ring, rep_weight(wd[ex], FF, D), aT, aT_t, [(0, TL)], epi_d)
            fps, fpt = P.next_psum()
            for c in range(KC):
                i = cnt[0] % 3
                cnt[0] += 1
                x, x_t = xb[i]
                P.dma("sp", x[:, 0:TL], xres[c * 128:(c + 1) * 128, g0:g0 + TL], reads=[xres_t[c]], writes=[x_t])
                P.stt("dve", acc[:, c, :], acc[:, c, :], md[:, 3 * KC + c:3 * KC + c + 1], x[:, 0:TL], ALU.mult, ALU.add,
                      reads=[acc_t, md_t, x_t], writes=[acc_t])
                sq, sq_t = sqs[c % 2]
                P.act(sq[:, 0:TL], acc[:, c, :], AF.Square, reads=[acc_t], writes=[sq_t])
                P.mm(fps[:, 0:TL], ones[:, :], sq[:, 0:TL], c == 0, c == KC - 1, reads=[ones_t, sq_t], writes=[fpt])
            P.ts("dve", rstd[:, 0:TL], fps[:, 0:TL], 1.0 / D, EPS, ALU.mult, ALU.add, reads=[fpt], writes=[rstd_t])
            P.act(rstd[:, 0:TL], rstd[:, 0:TL], AF.Sqrt, reads=[rstd_t], writes=[rstd_t])
            P.op("dve", lambda e: e.reciprocal(out=rstd[:, 0:TL], in_=rstd[:, 0:TL]), reads=[rstd_t], writes=[rstd_t])
            for c in range(KC):
                i = cnt[0] % 3
                cnt[0] += 1
                o, o_t = xo[i]
                P.stt("dve", o[:, 0:TL], acc[:, c, :], md[:, 5 * KC + c:5 * KC + c + 1], rstd[:, 0:TL], ALU.mult, ALU.mult,
                      reads=[acc_t, md_t, rstd_t], writes=[o_t])
                P.dma("sp", x2T[c * 128:(c + 1) * 128, g0:g0 + TL], o[:, 0:TL], reads=[o_t], is_out=True)
    P.emit()
    return nc


def build_l1in():
    NIN = 12304
    nc = new_nc()
    P = Prog(nc)
    P.alloc_psum(8)
    xT = din(nc, "xT", [D, TT])
    sc_in = din(nc, "sc", [128, KC])
    sh_in = din(nc, "sh", [128, KC])
    ng_in = din(nc, "ng", [128, KC])
    w_in = din(nc, "w_in", [D, NIN])
    w2_in = din(nc, "w2", [16, 2048])
    gb_in = din(nc, "gb", [128, 2048])
    qT = dout(nc, "qT", [2048, TT], BF16)
    kT = dout(nc, "kT", [2048, TT], BF16)
    vT = dout(nc, "vT", [4096, TT], BF16)
    rT = dout(nc, "rT", [4096, TT], BF16)
    la = dout(nc, "la", [TT, 2048])

    def ld(name, ap, shape):
        b, t = P.sb(name, shape, F32)
        P.dma("sp", b[:, :], ap, writes=[t])
        return b, t

    sc, sc_t = ld("sc_s", sc_in, [128, KC])
    sh, sh_t = ld("sh_s", sh_in, [128, KC])
    ng, ng_t = ld("ng_s", ng_in, [128, KC])
    w2, w2_t = ld("w2_s", w2_in, [16, 2048])
    gb, gb_t = ld("gb_s", gb_in, [128, 2048])
    ones, ones_t = P.sb("ones", [128, 128], F32)
    P.op("dve", lambda e: e.memset(ones[:, :], 1.0), writes=[ones_t])
    A, a_t = P.sb("A", [128, KC], F32)
    P.ts("dve", A[:, :], sc[:, :], 1.0, None, ALU.add, reads=[sc_t], writes=[a_t])
    P.tt("dve", A[:, :], A[:, :], ng[:, :], ALU.mult, reads=[a_t, ng_t], writes=[a_t])
    hT, h_t = P.sb("hT", [128, KC, TT], BF16)
    xt, xt_t = P.sb("xt", [128, KC, 256], F32)
    scr = P.sb("sq", [128, 256], F32) + P.sb("rstd", [128, 256], F32) + P.sb("tmpn", [128, 256], F32)
    P.op("act", lambda e: e.activation(out=sh[:, :], in_=sh[:, :], func=AF.Identity), reads=[sh_t, a_t], writes=[a_t, sh_t])
    norm_mod(P, xT, TT, A, sh, a_t, hT, h_t, 0, ones, ones_t, xt, xt_t, scr)
    ring = WRing(P, 3)
    gw = rep_weight(w_in, D, NIN)
    ob = [P.sb("ob%d" % i, [128, 512], BF16) for i in range(4)]
    obi = [0]
    qscale = 512.0 ** -0.5

    def epi(col0, tbi, tb, ps, pt):
        t0, tw = tb
        o, o_t = ob[obi[0] % 4]
        if col0 < 2048:
            dst, r0 = qT, col0
            P.ts("dve", o[:, 0:tw], ps[:, 0:tw], qscale, None, ALU.mult, reads=[pt], writes=[o_t])
        else:
            if col0 < 4096:
                dst, r0 = kT, col0 - 2048
            elif col0 < 8192:
                dst, r0 = vT, col0 - 4096
            else:
                dst, r0 = rT, col0 - 8192
            if obi[0] % 2 == 0:
                P.act(o[:, 0:tw], ps[:, 0:tw], AF.Copy, reads=[pt], writes=[o_t])
            else:
                P.op("dve", lambda e, o=o, ps=ps, tw=tw: e.tensor_copy(out=o[:, 0:tw], in_=ps[:, 0:tw]), reads=[pt], writes=[o_t])
        obi[0] += 1
        P.dma("sp", dst[r0:r0 + 128, t0:t0 + tw], o[:, 0:tw], reads=[o_t], is_out=True)

    gemm(P, ring, gw, hT, h_t, [(0, 512), (512, 512)], epi, 0, 12288)
    wg1, wg1_t = P.sb("wg1", [128, KC, 16], BF16)
    P.dma("pool", wg1[:, :, :], w_in.rearrange("(kc p) n -> p kc n", p=128)[:, :, 12288:12304], writes=[wg1_t])
    g1T, g1_t = P.sb("g1T", [16, TT], F32)
    for tb in range(2):
        ps, pt = P.next_psum()
        for kc in range(KC):
            P.mm(ps[0:16, :], wg1[:, kc, :], hT[:, kc, tb * 512:(tb + 1) * 512], kc == 0, kc == KC - 1,
                 reads=[wg1_t, h_t], writes=[pt])
        P.act(g1T[:, tb * 512:(tb + 1) * 512], ps[0:16, :], AF.Copy, reads=[pt, g1_t], writes=[g1_t])
    zt = [P.sb("zt%d" % i, [128, 512], F32) for i in range(2)]
    lo = [P.sb("lo%d" % i, [128, 512], F32) for i in range(2)]
    k = 0
    for tb in range(TT // 128):
        for cb in range(4):
            z, z_t = zt[k % 2]
            l, l_t = lo[k % 2]
            k += 1
            ps, pt = P.next_psum()
            P.mm(ps[:, :], g1T[:, tb * 128:(tb + 1) * 128], w2[:, cb * 512:(cb + 1) * 512], True, True,
                 reads=[g1_t, w2_t], writes=[pt])
            P.tt("dve", z[:, :], ps[:, :], gb[:, cb * 512:(cb + 1) * 512], ALU.add, reads=[pt, gb_t], writes=[z_t])
            P.act(z[:, :], z[:, :], AF.Exp, reads=[z_t], writes=[z_t], scale=-1.0)
            P.act(z[:, :], z[:, :], AF.Ln, reads=[z_t, ones_t], writes=[z_t], bias=ones[:, 0:1])
            P.ts("dve", l[:, :], z[:, :], -1.0 / 16.0, None, ALU.mult, reads=[z_t], writes=[l_t])
            P.dma("sp", la[tb * 128:(tb + 1) * 128, cb * 512:(cb + 1) * 512], l[:, :], reads=[l_t], is_out=True)
    P.emit()
    return nc


def build_gla():
    S = 4096
    DK = 512
    DV = 1024
    nc = new_nc()
    P = Prog(nc)
    P.alloc_psum(8)
    qT_in = din(nc, "qT", [DK, S], BF16)
    kT_in = din(nc, "kT", [DK, S], BF16)
    la_in = din(nc, "la", [S, DK])
    v_in = din(nc, "v", [S, DV], BF16)
    r_in = din(nc, "r", [S, DV], BF16)
    gn_in = din(nc, "gn", [128, DV])
    lt_in = din(nc, "lt2", [128, 128])
    mk_in = din(nc, "maskT", [64, 64])
    idb_in = din(nc, "identb", [128, 128], BF16)
    og = dout(nc, "og", [S, DV], BF16)
    qT, q_t = P.sb("qT_s", [128, 4, S], BF16)
    kT, k_t = P.sb("kT_s", [128, 4, S], BF16)
    for dc in range(4):
        P.dma("sp", qT[:, dc, :], qT_in[dc * 128:(dc + 1) * 128, :], reads=[q_t], writes=[q_t])
        P.dma("sp", kT[:, dc, :], kT_in[dc * 128:(dc + 1) * 128, :], reads=[k_t], writes=[k_t])
    gn, gn_t = P.sb("gn_s", [128, DV], F32)
    P.dma("sp", gn[:, :], gn_in, writes=[gn_t])
    lt2, lt_t = P.sb("lt2_s", [128, 128], F32)
    P.dma("sp", lt2[:, :], lt_in, writes=[lt_t])
    mk, mk_t = P.sb("mk_s", [64, 64], F32)
    P.dma("sp", mk[:, :], mk_in, writes=[mk_t])
    idb, idb_t = P.sb("idb_s", [128, 128], BF16)
    P.dma("sp", idb[:, :], idb_in, writes=[idb_t])
    S32, S_t = P.sb("S32", [128, 4, DV], F32)
    Sbf, Sbf_t = P.sb("Sbf", [128, 4, DV], BF16)
    S_ts = [[T() for _ in range(2)] for _ in range(4)]
    Sbf_ts = [[T() for _ in range(2)] for _ in range(4)]
    P.op("dve", lambda e: e.memset(S32[:, :, :], 0.0), writes=[S_t] + [t for r in S_ts for t in r])
    P.op("dve", lambda e: e.memset(Sbf[:, :, :], 0.0), writes=[Sbf_t] + [t for r in Sbf_ts for t in r])
    la_b = [P.sb("la%d" % i, [128, DK], F32) for i in range(2)]
    ebs = [P.sb("eb%d" % i, [128, 4, 128], F32) for i in range(2)]
    enbs = [P.sb("enb%d" % i, [128, 4, 128], F32) for i in range(2)]
    Qts = [P.sb("Qt%d" % i, [128, 4, 128], BF16) for i in range(2)]
    Kts = [P.sb("Kt%d" % i, [128, 4, 128], BF16) for i in range(2)]
    KdTs = [P.sb("KdT%d" % i, [128, 4, 64], BF16) for i in range(2)]
    Kds = [P.sb("Kd%d" % i, [64, DK], BF16) for i in range(2)]
    ats = [P.sb("at%d" % i, [64, 64], BF16) for i in range(2)]
    Vcs = [P.sb("Vc%d" % i, [64, DV], BF16) for i in range(3)]
    Rcs = [P.sb("Rc%d" % i, [64, DV], BF16) for i in range(2)]
    osbs = [P.sb("osb%d" % i, [64, DV], F32) for i in range(2)]
    junk, junk_t = P.sb("junk", [64, DV], F32)
    srs = [P.sb("sr%d" % i, [64, DV], F32) for i in range(2)]
    ons = [P.sb("on%d" % i, [64, DV], F32) for i in range(2)]
    ogts = [P.sb("ogt%d" % i, [64, DV], BF16) for i in range(2)]
    ssqs = [P.sb("ssq%d" % i, [64, 2], F32) for i in range(2)]
    ci = 0
    for pb in range(S // 128):
        t0 = pb * 128
        la, la_t = la_b[pb % 2]
        eb, eb_t = ebs[pb % 2]
        enb, enb_t = enbs[pb % 2]
        Qt, Qt_t = Qts[pb % 2]
        Kt, Kt_t = Kts[pb % 2]
        P.dma("sp", la[:, :], la_in[t0:t0 + 128, :], writes=[la_t])
        bps, bpt = P.next_psum()
        for dc in range(4):
            P.mm(bps[:, dc * 128:(dc + 1) * 128], la[:, dc * 128:(dc + 1) * 128], lt2[:, :], True, True,
                 reads=[la_t, lt_t], writes=[bpt])
        for dc in range(4):
            P.act(eb[:, dc, :], bps[:, dc * 128:(dc + 1) * 128], AF.Exp, reads=[bpt], writes=[eb_t])
            P.act(enb[:, dc, :], bps[:, dc * 128:(dc + 1) * 128], AF.Exp, reads=[bpt], writes=[enb_t], scale=-1.0)
            P.tt("dve", Qt[:, dc, :], qT[:, dc, t0:t0 + 128], eb[:, dc, :], ALU.mult, reads=[q_t, eb_t], writes=[Qt_t])
            P.tt("pool", Kt[:, dc, :], kT[:, dc, t0:t0 + 128], enb[:, dc, :], ALU.mult, reads=[k_t, enb_t], writes=[Kt_t])
        for c in range(2):
            tc = t0 + c * 64
            c0, c1 = c * 64, (c + 1) * 64
            Vc, Vc_t = Vcs[ci % 3]
            Rc, Rc_t = Rcs[ci % 2]
            KdT, KdT_t = KdTs[ci % 2]
            Kd, Kd_t = Kds[ci % 2]
            at, at_t = ats[ci % 2]
            osb, o_t = osbs[ci % 2]
            sr, sr_t = srs[ci % 2]
            on, on_t = ons[ci % 2]
            ogt, ogt_t = ogts[ci % 2]
            ssq, ssq_t = ssqs[ci % 2]
            ci += 1
            P.dma("sp", Vc[:, :], v_in[tc:tc + 64, :], writes=[Vc_t])
            P.dma("sp", Rc[:, :], r_in[tc:tc + 64, :], writes=[Rc_t])
            for dc in range(4):
                P.ts("dve", KdT[:, dc, :], Kt[:, dc, c0:c1], eb[:, dc, c1 - 1:c1], None, ALU.mult,
                     reads=[Kt_t, eb_t], writes=[KdT_t])
            aps, apt = P.next_psum()
            for dc in range(4):
                P.mm(aps[0:64, 0:64], Kt[:, dc, c0:c1], Qt[:, dc, c0:c1], dc == 0, dc == 3, reads=[Kt_t, Qt_t], writes=[apt])
            P.tt("dve", at[:, :], aps[0:64, 0:64], mk[:, :], ALU.mult, reads=[apt, mk_t], writes=[at_t])
            kps, kpt = P.next_psum()
            for dc in range(4):
                P.mm(kps[0:64, dc * 128:(dc + 1) * 128], KdT[:, dc, :], idb[:, :], True, True, reads=[KdT_t, idb_t], writes=[kpt])
            P.act(Kd[:, :], kps[0:64, :], AF.Copy, reads=[kpt], writes=[Kd_t])
            obanks = [P.next_psum() for _ in range(2)]
            for hf in range(2):
                ops, opt = obanks[hf]
                P.mm(ops[0:64, :], at[:, :], Vc[:, hf * 512:(hf + 1) * 512], True, False, reads=[at_t, Vc_t], writes=[opt])
                for dc in range(4):
                    P.mm(ops[0:64, :], Qt[:, dc, c0:c1], Sbf[:, dc, hf * 512:(hf + 1) * 512], False, dc == 3,
                         reads=[Qt_t, Sbf_ts[dc][hf]], writes=[opt])
            for hf in range(2):
                ops, opt = obanks[hf]
                P.act(osb[:, hf * 512:(hf + 1) * 512], ops[0:64, :], AF.Copy, reads=[opt], writes=[o_t])
            for dc in range(4):
                for hf in range(2):
                    sps, spt = P.next_psum()
                    P.mm(sps[:, :], Kd[:, dc * 128:(dc + 1) * 128], Vc[:, hf * 512:(hf + 1) * 512], True, True,
                         reads=[Kd_t, Vc_t], writes=[spt])
                    P.stt("dve", S32[:, dc, hf * 512:(hf + 1) * 512], S32[:, dc, hf * 512:(hf + 1) * 512], eb[:, dc, c1 - 1:c1],
                          sps[:, :], ALU.mult, ALU.add, reads=[S_ts[dc][hf], eb_t, spt], writes=[S_ts[dc][hf]])
                    P.op("pool", lambda e, dc=dc, hf=hf: e.tensor_copy(out=Sbf[:, dc, hf * 512:(hf + 1) * 512],
                                                                        in_=S32[:, dc, hf * 512:(hf + 1) * 512]),
                         reads=[S_ts[dc][hf]], writes=[Sbf_ts[dc][hf]])
            P.act(junk[:, :], osb[:, :], AF.Square, reads=[o_t], writes=[junk_t])
            P.op("dve", lambda e, ssq=ssq: e.reduce_sum(out=ssq[:, 0:1], in_=junk[:, :], axis=AX.X), reads=[junk_t], writes=[ssq_t])
            P.ts("dve", ssq[:, 0:1], ssq[:, 0:1], 1.0 / DV, EPS, ALU.mult, ALU.add, reads=[ssq_t], writes=[ssq_t])
            P.act(ssq[:, 0:1], ssq[:, 0:1], AF.Sqrt, reads=[ssq_t], writes=[ssq_t])
            P.op("dve", lambda e, ssq=ssq: e.reciprocal(out=ssq[:, 0:1], in_=ssq[:, 0:1]), reads=[ssq_t], writes=[ssq_t])
            P.act(sr[:, :], Rc[:, :], AF.Silu, reads=[Rc_t], writes=[sr_t])
            P.stt("dve", on[:, :], osb[:, :], ssq[:, 0:1], gn[0:64, :], ALU.mult, ALU.mult, reads=[o_t, ssq_t, gn_t], writes=[on_t])
            P.tt("pool", ogt[:, :], on[:, :], sr[:, :], ALU.mult, reads=[on_t, sr_t], writes=[ogt_t])
            P.dma("pool", og[tc:tc + 64, :], ogt[:, :], reads=[ogt_t], is_out=True)
    P.emit()
    return nc


def _fm(v):
    return np.ascontiguousarray(np.asarray(v, np.float32).reshape(32, 128).T)


def _rope_tables(t0):
    d = 128
    inv = 10000.0 ** (-np.arange(0, d, 2, dtype=np.float32) / d)
    pos = np.arange(t0, t0 + TT, dtype=np.float32)
    ang = pos[:, None] * inv[None, :]
    cos = np.cos(ang).astype(np.float32).T
    sin = np.sin(ang).astype(np.float32).T
    return np.ascontiguousarray(np.concatenate([cos, cos], 0)), np.ascontiguousarray(np.concatenate([sin, sin], 0))


def _rotm():
    R = np.zeros((128, 128), np.float32)
    for d in range(64):
        R[d + 64, d] = -1.0
        R[d, d + 64] = 1.0
    return R


def _diag_masks():
    m = np.zeros((4, 128, 512), np.float32)
    j = np.arange(128)[:, None]
    i = np.arange(512)[None, :]
    for o in range(4):
        m[o] = (i >= j + o * 128).astype(np.float32)
    return m


def _gla_consts():
    j = np.arange(128)[:, None]
    i = np.arange(128)[None, :]
    lt2 = ((j <= i) & (j // 64 == i // 64)).astype(np.float32)
    mk = (np.arange(64)[:, None] <= np.arange(64)[None, :]).astype(np.float32)
    return np.ascontiguousarray(lt2), np.ascontiguousarray(mk), np.eye(128, dtype=np.float32).astype(NPBF)


def _run(nc, in_maps):
    return run_bass_kernel_spmd(nc, in_maps, core_ids=list(range(NC))).results


def _c(a):
    return np.ascontiguousarray(np.asarray(a, np.float32))


def kernel(x, c, norm_gains, ada_w, ada_b, e_w_in, e_conv_w, e_conv_b, e_conv_ln_g, e_conv_ln_b,
           e_diff_lambda, e_diff_subln, e_w_out, e_ffn_gate, e_ffn_up, e_ffn_down, o_w_in, o_gate_w2,
           o_gate_b, o_gla_norm, o_w_out, o_router, o_exp_gate, o_exp_up, o_exp_down, final_norm):
    x = np.asarray(x, np.float32)
    B, S, _ = x.shape
    cT = np.ascontiguousarray(np.asarray(c).T.reshape(32, 128, 2).transpose(1, 0, 2).reshape(128, 64))
    ims = []
    for r in range(NC):
        sl = np.stack([np.asarray(ada_w[l])[:, w * D + r * 512:w * D + (r + 1) * 512] for l in range(2) for w in range(6)])
        ab = np.stack([np.asarray(ada_b[l])[w * D + r * 512:w * D + (r + 1) * 512].reshape(4, 128).T
                       for l in range(2) for w in range(6)], axis=1).reshape(128, 48)
        ims.append({"cT": cT, "ada_s": np.ascontiguousarray(sl), "adab": np.ascontiguousarray(ab)})
    res = _run(build_ada(), ims)
    del ims
    modp = np.stack([res[r]["modp"].reshape(128, 12, 4, 2) for r in range(NC)], axis=2)
    modp = modp.reshape(128, 12, 32, 2)

    def mod(l, w, b):
        return np.ascontiguousarray(modp[:, l * 6 + w, :, b])

    fin = _fm(final_norm)
    w0 = _c(e_w_in[0])
    cwT = np.ascontiguousarray(np.asarray(e_conv_w[0]).T.reshape(16, 128, 31).transpose(1, 0, 2).reshape(128, 16 * 31))
    cp = np.ascontiguousarray(np.concatenate([np.asarray(e_conv_b[0]).reshape(16, 128).T,
                                              np.asarray(e_conv_ln_g[0]).reshape(16, 128).T,
                                              np.asarray(e_conv_ln_b[0]).reshape(16, 128).T], axis=1).astype(np.float32))
    rotm = _rotm()
    ims = []
    for r in range(NC):
        b, j = r // 4, r % 4
        t0 = j * TT
        xs = np.zeros((TH, D), np.float32)
        if j > 0:
            xs[:] = x[b, t0 - HALO:t0 + TT]
        else:
            xs[HALO:] = x[b, 0:TT]
        cs, sn = _rope_tables(t0)
        ims.append({"xT": np.ascontiguousarray(xs.T), "sc": mod(0, 1, b), "sh": mod(0, 0, b), "ng": _fm(norm_gains[0, 0]),
                    "cw": cwT, "cp": cp, "halo": np.full((128, 1), 0.0 if j == 0 else 1.0, np.float32),
                    "cos": cs, "sin": sn, "rotm": rotm, "w_in": w0})
    res = _run(build_l0in(), ims)
    del ims, w0
    qT = [np.concatenate([res[b * 4 + j]["qT"] for j in range(4)], axis=1) for b in range(B)]
    kT = [np.concatenate([res[b * 4 + j]["kT"] for j in range(4)], axis=1) for b in range(B)]
    vT = [np.concatenate([res[b * 4 + j]["vT"] for j in range(4)], axis=1) for b in range(B)]
    yaT = [res[r]["yaT"] for r in range(NC)]
    lam = np.ascontiguousarray(np.asarray(e_diff_lambda[0], np.float32).reshape(1, 512))
    sub = np.ascontiguousarray(np.tile(np.asarray(e_diff_subln[0], np.float32)[None], (128, 1)))
    msk = _diag_masks()
    ims = []
    for r in range(NC):
        b, hp = r // 4, r % 4
        ims.append({"qT": np.ascontiguousarray(qT[b][hp * 512:(hp + 1) * 512]),
                    "kT": np.ascontiguousarray(kT[b][hp * 512:(hp + 1) * 512]),
                    "v": np.ascontiguousarray(vT[b][hp * 512:(hp + 1) * 512].T),
                    "lam": lam, "subln": sub, "mask": msk})
    res = _run(build_attn(), ims)
    del ims
    yb = [np.concatenate([res[b * 4 + hp]["yb"] for hp in range(4)], axis=1) for b in range(B)]
    wo, wg, wu, wd = _c(e_w_out[0]), _c(e_ffn_gate[0]), _c(e_ffn_up[0]), _c(e_ffn_down[0])
    ims = []
    for r in range(NC):
        b, j = r // 4, r % 4
        t0 = j * TT
        ybT = np.ascontiguousarray(yb[b][t0:t0 + TT].T).astype(NPBF)
        md = np.concatenate([mod(0, 2, b), mod(0, 3, b), mod(0, 4, b), mod(0, 5, b), _fm(norm_gains[0, 1]), fin], axis=1)
        ims.append({"xT": np.ascontiguousarray(x[b, t0:t0 + TT].T), "yT": np.ascontiguousarray(np.concatenate([yaT[r], ybT], axis=0)),
                    "md": np.ascontiguousarray(md), "w_out": wo, "w_gate": wg, "w_up": wu, "w_down": wd})
    res = _run(build_outffn(False, 11008), ims)
    del ims, wo, wg, wu, wd
    x1T = [res[r]["x2T"] for r in range(NC)]
    w1 = _c(o_w_in[0])
    w2 = _c(o_gate_w2[0])
    gb = np.ascontiguousarray(np.tile(np.asarray(o_gate_b[0], np.float32)[None], (128, 1)))
    ims = []
    for r in range(NC):
        b = r // 4
        ims.append({"xT": x1T[r], "sc": mod(1, 1, b), "sh": mod(1, 0, b), "ng": _fm(norm_gains[1, 0]),
                    "w_in": w1, "w2": w2, "gb": gb})
    res = _run(build_l1in(), ims)
    del ims, w1
    lt2, mk, idb = _gla_consts()
    gn = np.ascontiguousarray(np.tile(np.asarray(o_gla_norm[0], np.float32)[None], (128, 1)))
    cat = lambda name, b, ax: np.concatenate([res[b * 4 + j][name] for j in range(4)], axis=ax)
    ims = []
    for b in range(B):
        qb, kb, vb, rb, lab = cat("qT", b, 1), cat("kT", b, 1), cat("vT", b, 1), cat("rT", b, 1), cat("la", b, 0)
        for h in range(4):
            ims.append({"qT": np.ascontiguousarray(qb[h * 512:(h + 1) * 512]), "kT": np.ascontiguousarray(kb[h * 512:(h + 1) * 512]),
                        "la": np.ascontiguousarray(lab[:, h * 512:(h + 1) * 512]),
                        "v": np.ascontiguousarray(vb[h * 1024:(h + 1) * 1024].T), "r": np.ascontiguousarray(rb[h * 1024:(h + 1) * 1024].T),
                        "gn": gn, "lt2": lt2, "maskT": mk, "identb": idb})
    res = _run(build_gla(), ims)
    del ims
    wo, wr = _c(o_w_out[0]), _c(o_router[0])
    eg, eu, ed = _c(o_exp_gate[0]), _c(o_exp_up[0]), _c(o_exp_down[0])
    ident = np.eye(128, dtype=np.float32)
    ims = []
    for r in range(NC):
        b, j = r // 4, r % 4
        yT = np.ascontiguousarray(np.concatenate([res[b * 4 + h]["og"][j * TT:(j + 1) * TT].T for h in range(4)], axis=0))
        md = np.concatenate([mod(1, 2, b), mod(1, 3, b), mod(1, 4, b), mod(1, 5, b), _fm(norm_gains[1, 1]), fin], axis=1)
        ims.append({"xT": x1T[r], "yT": yT, "md": np.ascontiguousarray(md), "w_out": wo, "wr": wr, "ident": ident,
                    "w_gate": eg, "w_up": eu, "w_down": ed})
    res = _run(build_outffn(True, 4096), ims)
    out = np.zeros((B, S, D), np.float32)
    for r in range(NC):
        b, j = r // 4, r % 4
        out[b, j * TT:(j + 1) * TT] = res[r]["x2T"].T
    return out
```
